# Optimizing a Trainium2 kernel written in Bass

```python
import math
import jax, jax.numpy as jnp
from jax import lax
import numpy as np

D_MODEL = 2048
BATCH = 8
SEQ = 2048
DEPTH = 4

GRID_W = 64
GDN_HEADS = 6
GDN_DK = 128
GDN_DV = 128
GDN_WIDTH = GDN_HEADS * GDN_DV
SSM_HEADS = 12
SSM_HEADDIM = 64
SSM_WIDTH = SSM_HEADS * SSM_HEADDIM
SSM_GROUPS = 2
SSM_HPG = SSM_HEADS // SSM_GROUPS
SSM_STATE = 128
NA_HEADS = 8
NA_HEADDIM = 64
NA_WIDTH = NA_HEADS * NA_HEADDIM
NA_KH = 8
NA_KW = 16
MIX_WIDTH = GDN_WIDTH + SSM_WIDTH + NA_WIDTH
CONV_K = 5
CHUNK = 64
D_FF = 5632
N_EXPERTS = 8
TOP_K = 2
EPS = 1e-6

CONV_SIZES = (GDN_WIDTH, GDN_WIDTH, GDN_WIDTH, SSM_WIDTH, SSM_GROUPS * SSM_STATE, SSM_GROUPS * SSM_STATE)
REST_SIZES = (GDN_WIDTH, 2 * GDN_HEADS, 2 * GDN_HEADS, SSM_WIDTH, 2 * SSM_HEADS, NA_WIDTH, NA_WIDTH, NA_WIDTH)
CONV_CH = GDN_WIDTH * 3 + SSM_WIDTH + 2 * SSM_GROUPS * SSM_STATE
IN_WIDTH = CONV_CH + 2 * GDN_WIDTH + 4 * GDN_HEADS + 2 * SSM_HEADS + 3 * NA_WIDTH - GDN_WIDTH + SSM_WIDTH

kernel_name = "hybrid_parallel_heads_encoder"


def rmsnorm(x, g):
    xf = x.astype(jnp.float32)
    y = xf * lax.rsqrt(jnp.mean(xf * xf, axis=-1, keepdims=True) + EPS)
    return (y * g.astype(jnp.float32)).astype(x.dtype)


def l2norm(x):
    return x * lax.rsqrt(jnp.sum(x * x, axis=-1, keepdims=True) + EPS)


def split_points(sizes):
    return tuple(int(s) for s in np.cumsum(sizes)[:-1])


def depthwise_conv_silu(x, w, b):
    ch = x.shape[-1]
    y = lax.conv_general_dilated(x, w[:, None, :].astype(x.dtype), window_strides=(1,),
                                 padding=[(CONV_K // 2, CONV_K // 2)],
                                 dimension_numbers=('NWC', 'WIO', 'NWC'), feature_group_count=ch)
    return jax.nn.silu(y + b)


def gdn_one_direction(q, k, v, g, beta):
    bsz, L, H, DK = q.shape
    DV = v.shape[-1]
    nc = L // CHUNK

    def chunks(t):
        t = t.reshape((bsz, nc, CHUNK, H) + t.shape[3:])
        return jnp.moveaxis(t, 3, 1)

    q, k, v, beta = chunks(q), chunks(k), chunks(v), chunks(beta)
    g = jnp.cumsum(chunks(g), axis=-1)
    tri_incl = jnp.tril(jnp.ones((CHUNK, CHUNK), dtype=bool))
    tri_strict = jnp.tril(jnp.ones((CHUNK, CHUNK), dtype=bool), -1)
    decay = jnp.exp(jnp.where(tri_incl, g[..., :, None] - g[..., None, :], -jnp.inf))
    kk = jnp.einsum('bhnck,bhnsk->bhncs', k, k)
    a_mat = jnp.where(tri_strict, beta[..., None] * kk * decay, 0.0)
    ia = a_mat + jnp.eye(CHUNK, dtype=q.dtype)
    rhs = jnp.concatenate([v * beta[..., None], k * (beta * jnp.exp(g))[..., None]], axis=-1)
    sol = lax.linalg.triangular_solve(ia, rhs, left_side=True, lower=True, unit_diagonal=True)
    value, kcd = sol[..., :DV], sol[..., DV:]
    qk = jnp.einsum('bhnck,bhnsk->bhncs', q, k) * decay
    q_dec = q * jnp.exp(g)[..., None]
    g_last = g[..., -1]
    k_dec = k * jnp.exp(g_last[..., None] - g)[..., None]

    def step(S, xs):
        value_c, kcd_c, qk_c, q_dec_c, k_dec_c, gl_c = xs
        v_new = value_c - jnp.einsum('bhck,bhkv->bhcv', kcd_c, S)
        o = jnp.einsum('bhck,bhkv->bhcv', q_dec_c, S) + jnp.einsum('bhcs,bhsv->bhcv', qk_c, v_new)
        S = S * jnp.exp(gl_c)[..., None, None] + jnp.einsum('bhck,bhcv->bhkv', k_dec_c, v_new)
        return S, o

    xs = tuple(jnp.moveaxis(t, 2, 0) for t in (value, kcd, qk, q_dec, k_dec, g_last))
    s0 = jnp.zeros((bsz, H, DK, DV), q.dtype)
    _, o = lax.scan(step, s0, xs)
    o = jnp.moveaxis(o, 0, 2)
    return jnp.moveaxis(o, 1, 3).reshape(bsz, L, H, DV)


def gdn_mixer(qc, kc, vc, z, b_raw, a_raw, A_log, dt_bias, norm_w):
    bsz, L, _ = qc.shape
    f32 = jnp.float32
    q = l2norm(qc.astype(f32).reshape(bsz, L, GDN_HEADS, GDN_DK)) * (GDN_DK ** -0.5)
    k = l2norm(kc.astype(f32).reshape(bsz, L, GDN_HEADS, GDN_DK))
    v = vc.astype(f32).reshape(bsz, L, GDN_HEADS, GDN_DV)
    beta = jax.nn.sigmoid(b_raw.astype(f32).reshape(bsz, L, 2, GDN_HEADS))
    g = -jnp.exp(A_log.astype(f32)) * jax.nn.softplus(
        a_raw.astype(f32).reshape(bsz, L, 2, GDN_HEADS) + dt_bias.astype(f32))
    flip = lambda t: jnp.flip(t, axis=1)
    o_f = gdn_one_direction(q, k, v, g[:, :, 0], beta[:, :, 0])
    o_b = flip(gdn_one_direction(flip(q), flip(k), flip(v), flip(g[:, :, 1]), flip(beta[:, :, 1])))
    o = rmsnorm(o_f + o_b, norm_w) * jax.nn.silu(z.astype(f32).reshape(bsz, L, GDN_HEADS, GDN_DV))
    return o.reshape(bsz, L, GDN_WIDTH).astype(qc.dtype)


def ssd_one_direction(x, dt, A, Bm, Cm):
    bsz, L = x.shape[:2]
    nc = L // CHUNK
    ch = lambda t: t.reshape((bsz, nc, CHUNK) + t.shape[2:])
    xdt = ch(x * dt[..., None])
    a = jnp.moveaxis(ch(dt * A), 2, -1)
    Bm, Cm = ch(Bm), ch(Cm)
    acum = jnp.cumsum(a, axis=-1)
    tri_incl = jnp.tril(jnp.ones((CHUNK, CHUNK), dtype=bool))
    lmat = jnp.exp(jnp.where(tri_incl, acum[..., :, None] - acum[..., None, :], -jnp.inf))
    cb = jnp.einsum('bclgn,bcsgn->bcgls', Cm, Bm)
    y_diag = jnp.einsum('bcghls,bcsghp->bclghp', cb[:, :, :, None] * lmat, xdt)
    decay_states = jnp.moveaxis(jnp.exp(acum[..., -1:] - acum), -1, 2)
    states = jnp.einsum('bcsgn,bcsghp->bcghpn', Bm, xdt * decay_states[..., None])
    chunk_decay = jnp.exp(acum[..., -1])

    def step(h, inp):
        st, dc = inp
        return h * dc[..., None, None] + st, h

    h0 = jnp.zeros(states.shape[:1] + states.shape[2:], x.dtype)
    _, prev = lax.scan(step, h0, (jnp.moveaxis(states, 1, 0), jnp.moveaxis(chunk_decay, 1, 0)))
    prev = jnp.moveaxis(prev, 0, 1)
    y_off = jnp.einsum('bclgn,bcghpn->bclghp', Cm, prev) * jnp.exp(jnp.moveaxis(acum, -1, 2))[..., None]
    return (y_diag + y_off).reshape(x.shape)


def ssd_mixer(xc, Bc, Cc, z, dt_raw, A_log, dt_bias, d_skip, norm_w):
    bsz, L, _ = xc.shape
    f32 = jnp.float32
    x = xc.astype(f32).reshape(bsz, L, SSM_GROUPS, SSM_HPG, SSM_HEADDIM)
    Bm = Bc.astype(f32).reshape(bsz, L, SSM_GROUPS, SSM_STATE)
    Cm = Cc.astype(f32).reshape(bsz, L, SSM_GROUPS, SSM_STATE)
    dt = jax.nn.softplus(dt_raw.astype(f32).reshape(bsz, L, 2, SSM_HEADS) + dt_bias.astype(f32))
    dt = dt.reshape(bsz, L, 2, SSM_GROUPS, SSM_HPG)
    A = -jnp.exp(A_log.astype(f32)).reshape(2, SSM_GROUPS, SSM_HPG)
    flip = lambda t: jnp.flip(t, axis=1)
    y_f = ssd_one_direction(x, dt[:, :, 0], A[0], Bm, Cm)
    y_b = flip(ssd_one_direction(flip(x), flip(dt[:, :, 1]), A[1], flip(Bm), flip(Cm)))
    y = y_f + y_b + x * d_skip.astype(f32).reshape(SSM_GROUPS, SSM_HPG)[..., None]
    y = y.reshape(bsz, L, SSM_WIDTH) * jax.nn.silu(z.astype(f32))
    return rmsnorm(y, norm_w).astype(xc.dtype)


def na_mixer(q, k, v, rpb):
    bsz, L, _ = q.shape
    rows = L // GRID_W
    kh = min(NA_KH, rows)
    r = np.arange(rows)
    row_start = np.clip(r - kh // 2, 0, rows - kh)
    row_idx = row_start[:, None] + np.arange(kh)[None, :]
    dr_idx = (row_idx - r[:, None]) + NA_KH - 1
    cols = np.arange(GRID_W)
    col_start = np.clip(cols - NA_KW // 2, 0, GRID_W - NA_KW)
    in_win = (cols[None, :] >= col_start[:, None]) & (cols[None, :] < col_start[:, None] + NA_KW)
    dc_idx = np.clip(cols[None, :] - cols[:, None], -(NA_KW - 1), NA_KW - 1) + NA_KW - 1
    bias = rpb[:, dr_idx[:, None, :, None], dc_idx[None, :, None, :]].astype(jnp.float32)
    bias = jnp.where(in_win[None, None, :, None, :], bias, -jnp.inf)
    qg = q.reshape(bsz, rows, GRID_W, NA_HEADS, NA_HEADDIM)
    kg = k.reshape(bsz, rows, GRID_W, NA_HEADS, NA_HEADDIM)
    vg = v.reshape(bsz, rows, GRID_W, NA_HEADS, NA_HEADDIM)
    ridx = jnp.asarray(row_idx, dtype=jnp.int32)
    k_rows = jnp.take(kg, ridx, axis=1)
    v_rows = jnp.take(vg, ridx, axis=1)
    s = jnp.einsum('brqhd,brikhd->bhrqik', qg, k_rows).astype(jnp.float32) * (NA_HEADDIM ** -0.5) + bias[None]
    p = jax.nn.softmax(s.reshape(bsz, NA_HEADS, rows, GRID_W, kh * GRID_W), axis=-1)
    p = p.reshape(s.shape).astype(v.dtype)
    o = jnp.einsum('bhrqik,brikhd->brqhd', p, v_rows)
    return o.reshape(bsz, L, NA_WIDTH)


def swiglu(h, w1, w3, w2):
    return (jax.nn.silu(h @ w1) * (h @ w3)) @ w2


def moe_ffn(h, router, w1, w3, w2):
    bsz, L, D = h.shape
    t = h.reshape(-1, D)
    logits = (t @ router).astype(jnp.float32)
    top_val, top_idx = lax.top_k(logits, TOP_K)
    wts = jax.nn.softmax(top_val, axis=-1)
    gates = jnp.sum(jax.nn.one_hot(top_idx, N_EXPERTS, dtype=jnp.float32) * wts[..., None], axis=1).astype(t.dtype)
    out = jnp.zeros_like(t)
    for e in range(N_EXPERTS):
        out = out + gates[:, e:e + 1] * swiglu(t, w1[e], w3[e], w2[e])
    return out.reshape(bsz, L, D)


def setup_inputs(seed: int = 0) -> dict:
    key = jax.random.key(seed)
    ks = iter(jax.random.split(key, 40))
    f32 = jnp.float32
    nrm = lambda shape, s: jax.random.normal(next(ks), shape, f32) * s

    def a_log(shape):
        return jnp.log(jax.random.uniform(next(ks), shape, f32, 1.0, 16.0))

    def dt_bias(shape):
        dt = jnp.exp(jax.random.uniform(next(ks), shape, f32, math.log(1e-3), math.log(1e-1)))
        return dt + jnp.log(-jnp.expm1(-dt))

    n_dense = (DEPTH + 1) // 2
    n_moe = DEPTH // 2
    D = D_MODEL
    return {
        "x": nrm((BATCH, SEQ, D), 1.0),
        "c": nrm((BATCH, D), 1.0),
        "ada_w": nrm((DEPTH, D, 6 * D), 0.5 * D ** -0.5),
        "ada_b": nrm((DEPTH, 6 * D), 0.01),
        "norm_mix": 1.0 + nrm((DEPTH, D), 0.02),
        "norm_ffn": 1.0 + nrm((DEPTH, D), 0.02),
        "norm_final": 1.0 + nrm((D,), 0.02),
        "w_in": nrm((DEPTH, D, IN_WIDTH), D ** -0.5),
        "conv_w": nrm((DEPTH, CONV_K, CONV_CH), CONV_K ** -0.5),
        "conv_b": nrm((DEPTH, CONV_CH), 0.01),
        "gdn_A_log": a_log((DEPTH, 2, GDN_HEADS)),
        "gdn_dt_bias": dt_bias((DEPTH, 2, GDN_HEADS)),
        "gdn_norm": 1.0 + nrm((DEPTH, GDN_DV), 0.02),
        "ssm_A_log": a_log((DEPTH, 2, SSM_HEADS)),
        "ssm_dt_bias": dt_bias((DEPTH, 2, SSM_HEADS)),
        "ssm_D": 1.0 + nrm((DEPTH, SSM_HEADS), 0.1),
        "ssm_norm": 1.0 + nrm((DEPTH, SSM_WIDTH), 0.02),
        "na_rpb": nrm((DEPTH, NA_HEADS, 2 * NA_KH - 1, 2 * NA_KW - 1), 0.02),
        "w_out": nrm((DEPTH, MIX_WIDTH, D), MIX_WIDTH ** -0.5),
        "ffn_w1": nrm((n_dense, D, D_FF), D ** -0.5),
        "ffn_w3": nrm((n_dense, D, D_FF), D ** -0.5),
        "ffn_w2": nrm((n_dense, D_FF, D), D_FF ** -0.5),
        "moe_router": nrm((n_moe, D, N_EXPERTS), D ** -0.5),
        "moe_w1": nrm((n_moe, N_EXPERTS, D, D_FF), D ** -0.5),
        "moe_w3": nrm((n_moe, N_EXPERTS, D, D_FF), D ** -0.5),
        "moe_w2": nrm((n_moe, N_EXPERTS, D_FF, D), D_FF ** -0.5),
    }


def reference(x, c, ada_w, ada_b, norm_mix, norm_ffn, norm_final, w_in, conv_w, conv_b,
              gdn_A_log, gdn_dt_bias, gdn_norm, ssm_A_log, ssm_dt_bias, ssm_D, ssm_norm,
              na_rpb, w_out, ffn_w1, ffn_w3, ffn_w2, moe_router, moe_w1, moe_w3, moe_w2):
    conv_pts = split_points(CONV_SIZES)
    rest_pts = split_points(REST_SIZES)
    cond = jax.nn.silu(c)
    for l in range(DEPTH):
        mod = (cond @ ada_w[l] + ada_b[l])[:, None, :]
        sh1, sc1, g1, sh2, sc2, g2 = jnp.split(mod, 6, axis=-1)
        h = rmsnorm(x, norm_mix[l]) * (1.0 + sc1) + sh1
        proj = h @ w_in[l]
        conv_part = depthwise_conv_silu(proj[..., :CONV_CH], conv_w[l], conv_b[l])
        gq, gk, gv, sx, sB, sC = jnp.split(conv_part, conv_pts, axis=-1)
        gz, gb, ga, sz, sdt, nq, nk, nv = jnp.split(proj[..., CONV_CH:], rest_pts, axis=-1)
        ya = gdn_mixer(gq, gk, gv, gz, gb, ga, gdn_A_log[l], gdn_dt_bias[l], gdn_norm[l])
        yb = ssd_mixer(sx, sB, sC, sz, sdt, ssm_A_log[l], ssm_dt_bias[l], ssm_D[l], ssm_norm[l])
        yc = na_mixer(nq, nk, nv, na_rpb[l])
        y = jnp.concatenate([ya, yb, yc], axis=-1) @ w_out[l]
        x = x + g1 * y
        h2 = rmsnorm(x, norm_ffn[l]) * (1.0 + sc2) + sh2
        if l % 2 == 0:
            f = swiglu(h2, ffn_w1[l // 2], ffn_w3[l // 2], ffn_w2[l // 2])
        else:
            f = moe_ffn(h2, moe_router[l // 2], moe_w1[l // 2], moe_w3[l // 2], moe_w2[l // 2])
        x = x + g2 * f
    return rmsnorm(x, norm_final)
```

```python
import contextlib
import numpy as np
import concourse.bass as bass
import concourse.mybir as mybir
from concourse.bass_utils import run_bass_kernel_spmd

F32 = mybir.dt.float32
BF16 = mybir.dt.bfloat16
ALU = mybir.AluOpType
AF = mybir.ActivationFunctionType
AX = mybir.AxisListType

D = 2048
L = 2048
DEPTH = 4
NK = 16
IN_W = 6704
CONV_CH = 3584
DFF = 5632
NF = 44
NE = 8
EPS = 1e-6
CH = 128
NCK = L // CH
C_GQ, C_GK, C_GV, C_SX, C_SB, C_SC = 0, 768, 1536, 2304, 3072, 3328
C_GZ, C_GB, C_GA, C_SZ, C_SDT, C_NQ, C_NK, C_NV = 3584, 4352, 4364, 4376, 5144, 5168, 5680, 6192

N_DMA_SEMS = 6


class Buf:
    __slots__ = ("t", "w", "r", "name")

    def __init__(self, t, name=""):
        self.t = t
        self.w = {}
        self.r = {}
        self.name = name

    def __getitem__(self, idx):
        return self.t[idx]


class Sched:
    ENGS = ("pe", "act", "dve", "pool", "sp")

    def __init__(self, nc, stack, same_engine_sync=True):
        self.nc = nc
        self.stack = stack
        self.same = same_engine_sync
        self.sems = {}
        self.cnt = {}
        for e in self.ENGS:
            self._mksem(e)
        self.dq = {}
        for q in ("sp", "pool", "act"):
            keys = []
            for i in range(N_DMA_SEMS):
                k = f"d_{q}{i}"
                self._mksem(k)
                keys.append(k)
            self.dq[q] = [keys, 0]
        self.ops = {e: [] for e in self.ENGS}
        self.seen = {e: {} for e in self.ENGS}

    def _mksem(self, key):
        self.sems[key] = self.stack.enter_context(self.nc.semaphore(key))
        self.cnt[key] = 0

    def sbuf(self, name, shape, dt, st=None):
        self.uid = getattr(self, "uid", 0) + 1
        name = f"{name}_{self.uid}"
        t = (st or self.stack).enter_context(self.nc.sbuf_tensor(name, list(shape), dt))
        return Buf(t, name)

    def psum(self, name, shape, dt=F32):
        t = self.stack.enter_context(self.nc.psum_tensor(name, list(shape), dt))
        return Buf(t, name)

    def _deps(self, reads, writes):
        deps = {}
        for b in reads:
            for k, v in b.w.items():
                if deps.get(k, 0) < v:
                    deps[k] = v
        for b in writes:
            for d in (b.w, b.r):
                for k, v in d.items():
                    if deps.get(k, 0) < v:
                        deps[k] = v
        return deps

    def _emit_waits(self, eng, deps):
        seen = self.seen[eng]
        for k, v in deps.items():
            if k == eng and (not self.same or eng == "pe"):
                continue
            if seen.get(k, 0) >= v:
                continue
            seen[k] = v
            self.ops[eng].append(("wait", k, v))

    def op(self, eng, fn, reads=(), writes=()):
        deps = self._deps(reads, writes)
        self._emit_waits(eng, deps)
        self.cnt[eng] += 1
        v = self.cnt[eng]
        self.ops[eng].append(("op", fn, eng, 1))
        for b in reads:
            b.r[eng] = v
        for b in writes:
            b.w = {eng: v}
            b.r = {}

    def dma(self, q, fn, reads=(), writes=()):
        keys, idx = self.dq[q]
        k = keys[idx % len(keys)]
        self.dq[q][1] = idx + 1
        deps = self._deps(reads, writes)
        if self.cnt[k] > 0:
            deps[k] = max(deps.get(k, 0), self.cnt[k])
        self._emit_waits(q, deps)
        self.cnt[k] += 16
        v = self.cnt[k]
        self.ops[q].append(("op", fn, k, 16))
        for b in reads:
            b.r[k] = v
        for b in writes:
            b.w = {k: v}
            b.r = {}

    def barrier(self):
        for e in self.ENGS:
            deps = {k: v for k, v in self.cnt.items() if v > 0 and k != e}
            if self.same and e != "pe" and self.cnt[e] > 0:
                deps[e] = self.cnt[e]
            self._emit_waits(e, deps)

    def finish(self):
        self.barrier()
        nc = self.nc
        with nc.Block() as block:
            def mk(ename):
                def body(eng):
                    for item in self.ops[ename]:
                        if item[0] == "wait":
                            eng.wait_ge(self.sems[item[1]], item[2])
                        else:
                            _, fn, k, inc = item
                            fn(eng).then_inc(self.sems[k], inc)
                return body
            block.tensor(mk("pe"))
            block.scalar(mk("act"))
            block.vector(mk("dve"))
            block.gpsimd(mk("pool"))
            block.sync(mk("sp"))


def I(method, *a, **kw):
    return lambda e: getattr(e, method)(*a, **kw)


class Rot:
    def __init__(self, bufs):
        self.b = bufs
        self.i = 0

    def next(self):
        b = self.b[self.i % len(self.b)]
        self.i += 1
        return b


LAZY = {
    "ada_w": [DEPTH, D, 6 * D], "w_in": [DEPTH, D, IN_W], "w_out": [DEPTH, D, D],
    "ffn_w1": [2, D, DFF], "ffn_w3": [2, D, DFF], "ffn_w2": [2, DFF, D],
    "moe_router": [2, D, NE], "moe_w1": [2, NE, D, DFF], "moe_w3": [2, NE, D, DFF], "moe_w2": [2, NE, DFF, D],
    "na_rb": [DEPTH, 8, 64, 15 * 64],
}


class K:
    def __getattr__(self, name):
        if name in LAZY:
            b = Buf(self.nc.dram_tensor(name, list(LAZY[name]), F32, kind="ExternalInput").ap(), name)
            setattr(self, name, b)
            self.used.append(name)
            return b
        raise AttributeError(name)


def build(nlayers=DEPTH, flags="gsnf", dbg=()):
    nc = bass.Bass("TRN2", target_bir_lowering=False)
    k = K()
    k.nc = nc
    k.used = []
    k.flags = flags
    k.gstage = min([int(c) for c in flags if c.isdigit()] or [9])
    k.dbg = dbg

    def din(name, shape, dt=F32):
        return Buf(nc.dram_tensor(name, list(shape), dt, kind="ExternalInput").ap(), name)

    def dscr(name, shape, dt, out=False):
        kind = "ExternalOutput" if (out or name in dbg) else "Internal"
        return Buf(nc.dram_tensor(name, list(shape), dt, kind=kind).ap(), name)

    k.x = din("x", [L, D])
    k.cT = din("cT", [128, NK])
    k.ada_bT = din("ada_bT", [128, DEPTH, 96])
    k.nmixT = din("nmixT", [128, DEPTH, NK])
    k.nffnT = din("nffnT", [128, DEPTH, NK])
    k.nfinT = din("nfinT", [128, NK])
    k.conv_wT = din("conv_wT", [128, DEPTH, 28, 5])
    k.conv_bT = din("conv_bT", [128, DEPTH, 28])
    k.gate_bias = din("gate_bias", [128, DEPTH, 48])
    k.gate_alog = din("gate_alog", [128, DEPTH, 48])
    k.gdn_normT = din("gdn_normT", [128, DEPTH])
    k.ssm_Dbc = din("ssm_Dbc", [128, DEPTH, 12])
    k.ssm_normbc = din("ssm_normbc", [128, DEPTH, 768])
    k.c_identF = din("c_identF", [128, 128])
    k.c_mlow = din("c_mlow", [128, 128])
    k.c_mup = din("c_mup", [128, 128])
    k.c_namask = din("c_namask", [64, 64])
    k.c_sel = din("c_sel", [8, 8, 128])
    k.c_lvl = din("c_lvl", [128, 7, 128])
    k.out = dscr("out", [L, D], F32, out=True)
    k.xT = dscr("xT", [NK, 128, L], F32)
    k.mixT = dscr("mixT", [D, L], BF16)
    k.h2T = dscr("h2T", [NK, 128, L], BF16)
    k.yss = dscr("yss", [2, 128, NCK * 384], F32)
    if "modT" in dbg:
        k.dmod = dscr("modT", [128, DEPTH * 96], F32)
        k.dacc = dscr("accdbg", [128, NK * 1024], F32, out=True)
        k.dh2 = dscr("h2dump", [128, NK * L], BF16, out=True)

    with contextlib.ExitStack() as st:
        S = Sched(nc, st)
        k.S = S
        k.PS = [S.psum(f"ps{i}", [128, 512], F32) for i in range(8)]
        k.psrot = Rot(k.PS)
        k.identF = S.sbuf("identF", [128, 128], F32)
        k.identB = S.sbuf("identB", [128, 128], BF16)
        k.onesF = S.sbuf("onesF", [128, 128], F32)
        k.zerosF = S.sbuf("zerosF", [128, 128], F32)
        k.mlow = S.sbuf("mlow", [128, 128], F32)
        k.mup = S.sbuf("mup", [128, 128], F32)
        k.epsT = S.sbuf("epsT", [128, 1], F32)
        k.condT = S.sbuf("condT", [128, NK], F32)
        k.modT = S.sbuf("modT", [128, DEPTH, 96], F32)
        k.A1 = S.sbuf("A1", [128, DEPTH, NK], F32)
        k.A2 = S.sbuf("A2", [128, DEPTH, NK], F32)
        k.nfin = S.sbuf("nfin", [128, NK], F32)
        k.beta = S.sbuf("beta", [128, NCK, 12], F32)
        k.sp = S.sbuf("sp", [128, NCK, 36], F32)
        k.gdec = S.sbuf("gdec", [128, NCK, 36], F32)
        k.cw = S.sbuf("cw", [128, 28, 5], F32)
        k.cb = S.sbuf("cb", [128, 28], F32)
        k.hT = S.sbuf("hT", [128, NK, L], BF16)
        S.dma("sp", I("dma_start", out=k.identF[:], in_=k.c_identF[:]), [k.c_identF], [k.identF])
        S.dma("sp", I("dma_start", out=k.mlow[:], in_=k.c_mlow[:]), [k.c_mlow], [k.mlow])
        S.dma("sp", I("dma_start", out=k.mup[:], in_=k.c_mup[:]), [k.c_mup], [k.mup])
        S.dma("sp", I("dma_start", out=k.nfin[:], in_=k.nfinT[:]), [k.nfinT], [k.nfin])
        S.op("dve", I("tensor_copy", out=k.identB[:], in_=k.identF[:]), [k.identF], [k.identB])
        S.op("dve", I("memset", k.onesF[:], 1.0), [], [k.onesF])
        S.op("dve", I("memset", k.zerosF[:], 0.0), [], [k.zerosF])
        S.op("dve", I("memset", k.epsT[:], EPS), [], [k.epsT])

        phase0(k)
        for l in range(nlayers):
            phase_norm(k, l, 0)
            phase_mixers(k, l)
            phase_outproj(k, l)
            phase_norm(k, l, 1)
            phase_ffn(k, l)
        phase_final(k)
        S.finish()
    _CACHE["used"] = list(k.used)
    return nc


def mm(S, out, lhsT, rhs, start, stop, reads, writes):
    S.op("pe", I("matmul", out, lhsT, rhs, start=start, stop=stop), reads, writes)


def tr(S, out, in_, ident, reads, writes):
    S.op("pe", I("transpose", out, in_, ident), reads, writes)


def phase0(k):
    S = k.S
    with contextlib.ExitStack() as st:
        ada = Rot([S.sbuf(f"p0a{i}", [128, 6 * D], F32, st) for i in range(2)])
        xin = Rot([ada.b[0]])
        xo = Rot([ada.b[1]])
        nm = S.sbuf("p0nm", [128, DEPTH, NK], F32, st)
        nf = S.sbuf("p0nf", [128, DEPTH, NK], F32, st)
        adab = S.sbuf("p0ab", [128, DEPTH, 96], F32, st)
        for tt in range(16):
            xi = xin.next()
            o = xo.next()
            S.dma("sp", I("dma_start", out=xi[:, 0:D], in_=k.x[tt * 128:(tt + 1) * 128, :]), [k.x], [xi])
            for g in range(4):
                ps = k.psrot.next()
                for q in range(4):
                    kk = g * 4 + q
                    tr(S, ps[:, q * 128:(q + 1) * 128], xi[:, kk * 128:(kk + 1) * 128], k.identF[:], [xi, k.identF], [ps])
                src = ps[:].rearrange("p (q t) -> p q t", q=4)
                if g % 2 == 0:
                    S.op("dve", I("tensor_copy", out=o[:, 0:D].rearrange("p (k t) -> p k t", t=128)[:, g * 4:(g + 1) * 4, :], in_=src), [ps], [o])
                else:
                    S.op("act", I("activation", out=o[:, 0:D].rearrange("p (k t) -> p k t", t=128)[:, g * 4:(g + 1) * 4, :], in_=src, func=AF.Copy), [ps], [o])
            S.dma("sp", I("dma_start",
                out=k.xT[:, :, tt * 128:(tt + 1) * 128].rearrange("k p t -> p k t"), in_=o[:, 0:D].rearrange("p (k t) -> p k t", t=128)), [o], [k.xT])
        S.dma("sp", I("dma_start", out=k.condT[:], in_=k.cT[:]), [k.cT], [k.condT])
        S.op("act", I("activation", out=k.condT[:], in_=k.condT[:], func=AF.Silu), [k.condT], [k.condT])
        S.dma("sp", I("dma_start", out=adab[:], in_=k.ada_bT[:]), [k.ada_bT], [adab])
        S.dma("sp", I("dma_start", out=nm[:], in_=k.nmixT[:]), [k.nmixT], [nm])
        S.dma("sp", I("dma_start", out=nf[:], in_=k.nffnT[:]), [k.nffnT], [nf])
        for l in range(DEPTH):
            ps = k.psrot.next()
            mm(S, ps[:, 0:96], k.zerosF[:, :], k.zerosF[:, 0:96], True, False, [k.zerosF], [ps])
            for kk in range(NK):
                blk = ada.next()
                S.dma("sp", I("dma_start", out=blk[:], in_=k.ada_w[l, kk * 128:(kk + 1) * 128, :]), [k.ada_w], [blk])
                for j in range(96):
                    mm(S, ps[:, j:j + 1], blk[:, j * 128:(j + 1) * 128], k.condT[:, kk:kk + 1], False, kk == NK - 1, [blk, k.condT], [ps])
            S.op("dve", I("tensor_tensor", out=k.modT[:, l, :], in0=ps[:, 0:96], in1=adab[:, l, :], op=ALU.add), [ps, adab], [k.modT])
        S.op("dve", I("scalar_tensor_tensor", out=k.A1[:], in0=k.modT[:, :, 16:32], scalar=1.0, in1=nm[:], op0=ALU.add, op1=ALU.mult), [k.modT, nm], [k.A1])
        S.op("dve", I("scalar_tensor_tensor", out=k.A2[:], in0=k.modT[:, :, 64:80], scalar=1.0, in1=nf[:], op0=ALU.add, op1=ALU.mult), [k.modT, nf], [k.A2])
        S.barrier()


def phase_norm(k, l, which):
    S = k.S
    Asc = k.A1 if which == 0 else k.A2
    shoff = 0 if which == 0 else 48
    with contextlib.ExitStack() as st:
        xb = Rot([S.sbuf(f"nx{i}", [128, NK, 512], F32, st) for i in range(2)])
        sq = Rot([S.sbuf(f"nsq{i}", [128, 512], F32, st) for i in range(2)])
        tmp = Rot([S.sbuf(f"ntm{i}", [128, 512], F32, st) for i in range(2)])
        rstd = Rot([S.sbuf(f"nrs{i}", [128, 512], F32, st) for i in range(2)])
        for t in range(4):
            x = xb.next()
            S.dma("sp", I("dma_start", out=x[:], in_=k.xT[:, :, t * 512:(t + 1) * 512].rearrange("k p t -> p k t")), [k.xT], [x])
            ps = k.psrot.next()
            for kk in range(NK):
                s = sq.next()
                S.op("act", I("activation", out=s[:], in_=x[:, kk, :], func=AF.Square), [x], [s])
                mm(S, ps[:], k.onesF[:], s[:], kk == 0, kk == NK - 1, [k.onesF, s], [ps])
            r = rstd.next()
            S.op("act", I("activation", out=r[:], in_=ps[:], func=AF.Sqrt, bias=k.epsT[:], scale=1.0 / D), [ps, k.epsT], [r])
            S.op("dve", I("reciprocal", out=r[:], in_=r[:]), [r], [r])
            for kk in range(NK):
                tm = tmp.next()
                S.op("dve", I("tensor_tensor", out=tm[:], in0=x[:, kk, :], in1=r[:], op=ALU.mult), [x, r], [tm])
                S.op("act", I("activation",
                    out=k.hT[:, kk, t * 512:(t + 1) * 512], in_=tm[:], func=AF.Identity,
                    bias=k.modT[:, l, shoff + kk:shoff + kk + 1], scale=Asc[:, l, kk:kk + 1]), [tm, k.modT, Asc], [k.hT])
        S.barrier()
    if "hT" in k.dbg and which == 0 and l == 0 or "h2dbg" in k.dbg and which == 1 and l == 0:
        S.dma("sp", I("dma_start", out=k.h2T[:].rearrange("k p t -> p k t"), in_=k.hT[:]), [k.hT], [k.h2T])
        S.barrier()


def phase_outproj(k, l):
    S = k.S
    with contextlib.ExitStack() as st:
        wr = Rot([S.sbuf(f"opw{i}", [128, NK, 128], BF16, st) for i in range(2)])
        xr = Rot([S.sbuf(f"opx{i}", [128, 512], F32, st) for i in range(3)])
        for m in range(NK):
            S.dma("sp", I("dma_start", out=k.hT[:, m, :], in_=k.mixT[m * 128:(m + 1) * 128, :]), [k.mixT], [k.hT])
        for j in range(NK):
            w = wr.next()
            S.dma("pool", I("dma_start",
                out=w[:], in_=k.w_out[l, :, j * 128:(j + 1) * 128].rearrange("(m p) c -> p m c", p=128)), [k.w_out], [w])
            for t in range(4):
                ps = k.psrot.next()
                for m in range(NK):
                    mm(S, ps[:], w[:, m, :], k.hT[:, m, t * 512:(t + 1) * 512], m == 0, m == NK - 1, [w, k.hT], [ps])
                xt = xr.next()
                S.dma("sp", I("dma_start", out=xt[:], in_=k.xT[j, :, t * 512:(t + 1) * 512]), [k.xT], [xt])
                S.op("dve", I("scalar_tensor_tensor",
                    out=xt[:], in0=ps[:], scalar=k.modT[:, l, 32 + j:33 + j], in1=xt[:], op0=ALU.mult, op1=ALU.add), [ps, xt, k.modT], [xt])
                S.dma("sp", I("dma_start", out=k.xT[j, :, t * 512:(t + 1) * 512], in_=xt[:]), [xt], [k.xT])
        S.barrier()


def phase_ffn(k, l):
    S = k.S
    moe = (l % 2 == 1)
    li = l // 2
    TH = 1024
    NQ = 11
    FQ = NF // NQ
    with contextlib.ExitStack() as st:
        acc = S.sbuf("facc", [128, NK, TH], F32, st)
        G = S.sbuf("fG", [128, FQ, TH], BF16, st)
        w1r = Rot([S.sbuf(f"fw1{i}", [128, NK, 128], BF16, st) for i in range(2)])
        w3r = Rot([S.sbuf(f"fw3{i}", [128, NK, 128], BF16, st) for i in range(2)])
        w2r = Rot([S.sbuf(f"fw2{i}", [128, FQ, 128], BF16, st) for i in range(2)])
        sar = Rot([S.sbuf(f"fsa{i}", [128, 512], F32, st) for i in range(2)])
        tmr = Rot([S.sbuf(f"ftm{i}", [128, 512], F32, st) for i in range(2)])
        xr = Rot([S.sbuf(f"fx{i}", [128, 512], F32, st) for i in range(2)])
        if moe:
            rt = S.sbuf("frt", [128, NK, NE], BF16, st)
            lg = S.sbuf("flg", [128, 8, NE], F32, st)
            gate = S.sbuf("fgate", [128, 8, NE], F32, st)
            m8 = S.sbuf("fm8", [128, 8], F32, st)
            sm = S.sbuf("fsm", [128, 4], F32, st)
            selm = S.sbuf("fselm", [128, NE], F32, st)
            ex = S.sbuf("fex", [128, NE], F32, st)
            gT = S.sbuf("fgT", [8, TH], F32, st)
            selT = S.sbuf("fselT", [8, NE, 128], F32, st)
            gbc = Rot([S.sbuf(f"fgbc{i}", [128, TH], F32, st) for i in range(2)])
            S.dma("pool", I("dma_start", out=rt[:], in_=k.moe_router[li].rearrange("(m p) c -> p m c", p=128)), [k.moe_router], [rt])
            S.dma("sp", I("dma_start", out=selT[:], in_=k.c_sel[:]), [k.c_sel], [selT])
        for half in range(2):
            t0 = half * TH
            if moe:
                ps = k.psrot.next()
                for tt in range(8):
                    for kk in range(NK):
                        mm(S, ps[:, tt * NE:(tt + 1) * NE], k.hT[:, kk, t0 + tt * 128:t0 + (tt + 1) * 128], rt[:, kk, :], kk == 0, kk == NK - 1, [k.hT, rt], [ps])
                S.op("dve", I("tensor_copy", out=lg[:], in_=ps[:, 0:8 * NE].rearrange("p (t e) -> p t e", e=NE)), [ps], [lg])
                psT = k.psrot.next()
                for tt in range(8):
                    S.op("dve", I("max", out=m8[:], in_=lg[:, tt, :]), [lg], [m8])
                    S.op("dve", I("tensor_scalar", out=selm[:], in0=lg[:, tt, :], scalar1=m8[:, 1:2], scalar2=None, op0=ALU.is_ge), [lg, m8], [selm])
                    S.op("dve", I("tensor_scalar", out=sm[:, 0:1], in0=m8[:, 0:1], scalar1=-1.0, scalar2=None, op0=ALU.mult), [m8], [sm])
                    S.op("act", I("activation", out=ex[:], in_=lg[:, tt, :], func=AF.Exp, bias=sm[:, 0:1], scale=1.0), [lg, sm], [ex])
                    S.op("dve", I("tensor_tensor", out=ex[:], in0=ex[:], in1=selm[:], op=ALU.mult), [ex, selm], [ex])
                    S.op("dve", I("reduce_sum", out=sm[:, 1:2], in_=ex[:], axis=AX.X), [ex], [sm])
                    S.op("dve", I("reciprocal", out=sm[:, 2:3], in_=sm[:, 1:2]), [sm], [sm])
                    S.op("dve", I("tensor_scalar", out=gate[:, tt, :], in0=ex[:], scalar1=sm[:, 2:3], scalar2=None, op0=ALU.mult), [ex, sm], [gate])
                    tr(S, psT[0:8, tt * 128:(tt + 1) * 128] if tt < 4 else psT[0:8, (tt - 4) * 128:(tt - 3) * 128], gate[:, tt, :], k.identF[:], [gate, k.identF], [psT])
                    if tt == 3 or tt == 7:
                        hh = 0 if tt == 3 else 1
                        S.op("dve", I("tensor_copy", out=gT[:, hh * 512:(hh + 1) * 512], in_=psT[0:8, :]), [psT], [gT])
                        if tt == 3:
                            psT = k.psrot.next()
            nexp = NE if moe else 1
            first = True
            for ex_i in range(nexp):
                if moe:
                    W1, W3, W2 = k.moe_w1[li, ex_i], k.moe_w3[li, ex_i], k.moe_w2[li, ex_i]
                    gb = gbc.next()
                    for t in range(2):
                        ps = k.psrot.next()
                        mm(S, ps[:], selT[:, ex_i, :], gT[:, t * 512:(t + 1) * 512], True, True, [selT, gT], [ps])
                        S.op("act", I("activation", out=gb[:, t * 512:(t + 1) * 512], in_=ps[:], func=AF.Copy), [ps], [gb])
                else:
                    W1, W3, W2 = k.ffn_w1[li], k.ffn_w3[li], k.ffn_w2[li]
                for q in range(NQ):
                    for fi in range(FQ):
                        f = q * FQ + fi
                        w1 = w1r.next()
                        w3 = w3r.next()
                        S.dma("pool", I("dma_start", out=w1[:], in_=W1[:, f * 128:(f + 1) * 128].rearrange("(m p) c -> p m c", p=128)), [k.moe_w1 if moe else k.ffn_w1], [w1])
                        S.dma("pool", I("dma_start", out=w3[:], in_=W3[:, f * 128:(f + 1) * 128].rearrange("(m p) c -> p m c", p=128)), [k.moe_w3 if moe else k.ffn_w3], [w3])
                        for t in range(2):
                            pa = k.psrot.next()
                            pb = k.psrot.next()
                            for kk in range(NK):
                                mm(S, pa[:], w1[:, kk, :], k.hT[:, kk, t0 + t * 512:t0 + (t + 1) * 512], kk == 0, kk == NK - 1, [w1, k.hT], [pa])
                            for kk in range(NK):
                                mm(S, pb[:], w3[:, kk, :], k.hT[:, kk, t0 + t * 512:t0 + (t + 1) * 512], kk == 0, kk == NK - 1, [w3, k.hT], [pb])
                            sa = sar.next()
                            S.op("act", I("activation", out=sa[:], in_=pa[:], func=AF.Silu), [pa], [sa])
                            S.op("dve", I("tensor_tensor", out=G[:, fi, t * 512:(t + 1) * 512], in0=pb[:], in1=sa[:], op=ALU.mult), [pb, sa], [G])
                    for j in range(NK):
                        w2 = w2r.next()
                        S.dma("pool", I("dma_start",
                            out=w2[:], in_=W2[q * FQ * 128:(q + 1) * FQ * 128, j * 128:(j + 1) * 128].rearrange("(i p) c -> p i c", p=128)), [k.moe_w2 if moe else k.ffn_w2], [w2])
                        for t in range(2):
                            ps = k.psrot.next()
                            for fi in range(FQ):
                                mm(S, ps[:], w2[:, fi, :], G[:, fi, t * 512:(t + 1) * 512], fi == 0, fi == FQ - 1, [w2, G], [ps])
                            dst = acc[:, j, t * 512:(t + 1) * 512]
                            if moe:
                                if first:
                                    S.op("dve", I("tensor_tensor", out=dst, in0=ps[:], in1=gb[:, t * 512:(t + 1) * 512], op=ALU.mult), [ps, gb], [acc])
                                else:
                                    tm = tmr.next()
                                    S.op("dve", I("tensor_tensor", out=tm[:], in0=ps[:], in1=gb[:, t * 512:(t + 1) * 512], op=ALU.mult), [ps, gb], [tm])
                                    S.op("pool", I("tensor_tensor", out=dst, in0=dst, in1=tm[:], op=ALU.add), [tm, acc], [acc])
                            else:
                                if first:
                                    S.op("act", I("activation", out=dst, in_=ps[:], func=AF.Copy), [ps], [acc])
                                else:
                                    S.op("dve", I("tensor_tensor", out=dst, in0=ps[:], in1=dst, op=ALU.add), [ps, acc], [acc])
                    first = False
            if "modT" in k.dbg and half == 0 and l == 0:
                S.dma("sp", I("dma_start", out=k.dacc[:], in_=acc[:].rearrange("p a b -> p (a b)")), [acc], [k.dacc])
                S.dma("sp", I("dma_start", out=k.dmod[:], in_=k.modT[:].rearrange("p a b -> p (a b)")), [k.modT], [k.dmod])
                S.dma("sp", I("dma_start", out=k.dh2[:], in_=k.hT[:].rearrange("p a b -> p (a b)")), [k.hT], [k.dh2])
            for j in range(NK):
                for t in range(2):
                    xt = xr.next()
                    S.dma("sp", I("dma_start", out=xt[:], in_=k.xT[j, :, t0 + t * 512:t0 + (t + 1) * 512]), [k.xT], [xt])
                    S.op("dve", I("scalar_tensor_tensor",
                        out=xt[:], in0=acc[:, j, t * 512:(t + 1) * 512], scalar=k.modT[:, l, 80 + j:81 + j], in1=xt[:], op0=ALU.mult, op1=ALU.add), [acc, xt, k.modT], [xt])
                    S.dma("sp", I("dma_start", out=k.xT[j, :, t0 + t * 512:t0 + (t + 1) * 512], in_=xt[:]), [xt], [k.xT])
        S.barrier()


def phase_final(k):
    S = k.S
    with contextlib.ExitStack() as st:
        xb = Rot([S.sbuf(f"zx{i}", [128, NK, 512], F32, st) for i in range(2)])
        sq = Rot([S.sbuf(f"zsq{i}", [128, 512], F32, st) for i in range(2)])
        rstd = S.sbuf("zrs", [128, 512], F32, st)
        ob = Rot([S.sbuf(f"zo{i}", [128, D], F32, st) for i in range(2)])
        for t in range(4):
            x = xb.next()
            S.dma("sp", I("dma_start", out=x[:], in_=k.xT[:, :, t * 512:(t + 1) * 512].rearrange("k p t -> p k t")), [k.xT], [x])
            ps = k.psrot.next()
            for kk in range(NK):
                s = sq.next()
                S.op("act", I("activation", out=s[:], in_=x[:, kk, :], func=AF.Square), [x], [s])
                mm(S, ps[:], k.onesF[:], s[:], kk == 0, kk == NK - 1, [k.onesF, s], [ps])
            S.op("act", I("activation", out=rstd[:], in_=ps[:], func=AF.Sqrt, bias=k.epsT[:], scale=1.0 / D), [ps, k.epsT], [rstd])
            S.op("dve", I("reciprocal", out=rstd[:], in_=rstd[:]), [rstd], [rstd])
            for kk in range(NK):
                S.op("dve", I("scalar_tensor_tensor",
                    out=x[:, kk, :], in0=x[:, kk, :], scalar=k.nfin[:, kk:kk + 1], in1=rstd[:], op0=ALU.mult, op1=ALU.mult), [x, k.nfin, rstd], [x])
            for tb in range(4):
                o = ob.next()
                for g in range(4):
                    ps2 = k.psrot.next()
                    for q in range(4):
                        kk = g * 4 + q
                        tr(S, ps2[:, q * 128:(q + 1) * 128], x[:, kk, tb * 128:(tb + 1) * 128], k.identF[:], [x, k.identF], [ps2])
                    if g % 2 == 0:
                        S.op("dve", I("tensor_copy", out=o[:, g * 512:(g + 1) * 512], in_=ps2[:]), [ps2], [o])
                    else:
                        S.op("act", I("activation", out=o[:, g * 512:(g + 1) * 512], in_=ps2[:], func=AF.Copy), [ps2], [o])
                r0 = t * 512 + tb * 128
                S.dma("sp", I("dma_start", out=k.out[r0:r0 + 128, :], in_=o[:]), [o], [k.out])
        S.barrier()


def evac(S, i, out, in_, reads, writes):
    if i % 2 == 0:
        S.op("dve", I("tensor_copy", out=out, in_=in_), reads, writes)
    else:
        S.op("act", I("activation", out=out, in_=in_, func=AF.Copy), reads, writes)


def phase_mixers(k, l):
    S = k.S
    with contextlib.ExitStack() as st:
        if not all(c in k.flags for c in "gsn"):
            z = S.sbuf("mxz", [128, L], BF16, st)
            S.op("dve", I("memset", z[:], 0.0), [], [z])
            for m in range(NK):
                S.dma("sp", I("dma_start", out=k.mixT[m * 128:(m + 1) * 128, :], in_=z[:]), [z], [k.mixT])
            S.barrier()
    if "g" in k.flags or "s" in k.flags:
        phase_gates(k, l)
    if "g" in k.flags:
        mixer_gdn(k, l)
    if "s" in k.flags:
        mixer_ssd(k, l)
    if "n" in k.flags:
        mixer_na(k, l)


def mixer_na(k, l):
    S = k.S
    rot6 = Rot(k.PS[0:6])
    accr = Rot(k.PS[6:8])
    with contextlib.ExitStack() as st:
        wv = S.sbuf("nawv", [128, NK, 512], BF16, st)
        vT = S.sbuf("navt", [64, 32, 512], BF16, st)
        wq = Rot([S.sbuf(f"nawq{i}", [128, NK, 64], BF16, st) for i in range(2)])
        qT = S.sbuf("naq", [64, L], BF16, st)
        kT = S.sbuf("nak", [64, L], BF16, st)
        rb = S.sbuf("narb", [64, 15 * 64], F32, st)
        msk = S.sbuf("namsk", [64, 64], F32, st)
        sbr = Rot([S.sbuf(f"nas{i}", [64, 512], F32, st) for i in range(3)])
        pbr = Rot([S.sbuf(f"nap{i}", [64, 512], BF16, st) for i in range(3)])
        ptr_ = Rot([S.sbuf(f"napt{i}", [64, 8, 64], BF16, st) for i in range(3)])
        smr = Rot([S.sbuf(f"nasm{i}", [64, 4], F32, st) for i in range(4)])
        yc = S.sbuf("nayc", [64, L], BF16, st)
        S.dma("pool", I("dma_start", out=wv[:], in_=k.w_in[l, :, C_NV:C_NV + 512].rearrange("(m p) c -> p m c", p=128)), [k.w_in], [wv])
        S.dma("sp", I("dma_start", out=msk[:], in_=k.c_namask[:]), [k.c_namask], [msk])
        for i in range(32):
            ps = rot6.next()
            for kk in range(NK):
                mm(S, ps[0:64, :], k.hT[:, kk, i * 64:(i + 1) * 64], wv[:, kk, :], kk == 0, kk == NK - 1, [k.hT, wv], [ps])
            evac(S, i, vT[:, i, :], ps[0:64, :], [ps], [vT])
        for h in range(8):
            for dst, c0 in ((qT, C_NQ + h * 64), (kT, C_NK + h * 64)):
                w = wq.next()
                S.dma("pool", I("dma_start", out=w[:], in_=k.w_in[l, :, c0:c0 + 64].rearrange("(m p) c -> p m c", p=128)), [k.w_in], [w])
                for t in range(4):
                    ps = rot6.next()
                    for kk in range(NK):
                        mm(S, ps[0:64, :], w[:, kk, :], k.hT[:, kk, t * 512:(t + 1) * 512], kk == 0, kk == NK - 1, [w, k.hT], [ps])
                    evac(S, t, dst[:, t * 512:(t + 1) * 512], ps[0:64, :], [ps], [dst])
            S.dma("sp", I("dma_start", out=rb[:], in_=k.na_rb[l, h]), [k.na_rb], [rb])
            S.op("dve", I("tensor_tensor", out=rb[:].rearrange("p (d c) -> p d c", c=64), in0=rb[:].rearrange("p (d c) -> p d c", c=64),
                                                  in1=msk[:].unsqueeze(1).broadcast_to([64, 15, 64]), op=ALU.add), [rb, msk], [rb])
            po = None
            for r in range(32):
                rs = min(max(r - 4, 0), 24)
                dr0 = rs - r + 7
                ps = rot6.next()
                mm(S, ps[0:64, :], qT[:, r * 64:(r + 1) * 64], kT[:, rs * 64:rs * 64 + 512], True, True, [qT, kT], [ps])
                s = sbr.next()
                sm = smr.next()
                S.op("dve", I("scalar_tensor_tensor",
                    out=s[:], in0=ps[0:64, :], scalar=0.125, in1=rb[:, dr0 * 64:dr0 * 64 + 512], op0=ALU.mult, op1=ALU.add), [ps, rb], [s])
                S.op("dve", I("reduce_max", out=sm[:, 0:1], in_=s[:], axis=AX.X), [s], [sm])
                S.op("dve", I("tensor_scalar", out=sm[:, 1:2], in0=sm[:, 0:1], scalar1=-1.0, scalar2=None, op0=ALU.mult), [sm], [sm])
                S.op("act", I("activation", out=s[:], in_=s[:], func=AF.Exp, bias=sm[:, 1:2], scale=1.0, accum_out=sm[:, 2:3]), [s, sm], [s, sm])
                S.op("dve", I("reciprocal", out=sm[:, 3:4], in_=sm[:, 2:3]), [sm], [sm])
                p_ = pbr.next()
                S.op("dve", I("tensor_scalar", out=p_[:], in0=s[:], scalar1=sm[:, 3:4], scalar2=None, op0=ALU.mult), [s, sm], [p_])
                pst = rot6.next()
                pstb = pst[:].bitcast(BF16)
                for i in range(8):
                    tr(S, pstb[0:64, i * 64:(i + 1) * 64], p_[:, i * 64:(i + 1) * 64], k.identB[0:64, 0:64], [p_, k.identB], [pst])
                pt = ptr_.next()
                S.op("act", I("activation", out=pt[:].rearrange("p a b -> p (a b)"), in_=pstb[0:64, 0:512], func=AF.Copy), [pst], [pt])
                if r % 8 == 0:
                    po = accr.next()
                for i in range(8):
                    mm(S, po[0:64, (r % 8) * 64:(r % 8 + 1) * 64], vT[:, rs + i, h * 64:(h + 1) * 64], pt[:, i, :], i == 0, i == 7, [vT, pt], [po])
                if r % 8 == 7:
                    evac(S, r // 8, yc[:, (r - 7) * 64:(r + 1) * 64], po[0:64, :], [po], [yc])
            S.dma("sp", I("dma_start", out=k.mixT[1536 + h * 64:1536 + (h + 1) * 64, :], in_=yc[:]), [yc], [k.mixT])
        S.barrier()


def phase_gates(k, l):
    S = k.S
    with contextlib.ExitStack() as st:
        wg = S.sbuf("gwg", [128, NK, 48], BF16, st)
        GT = S.sbuf("gGT", [128, NCK, 48], F32, st)
        gbias = S.sbuf("ggb", [128, 48], F32, st)
        galog = S.sbuf("gga", [128, 48], F32, st)
        S.dma("pool", I("dma_start", out=wg[:, :, 0:24], in_=k.w_in[l, :, C_GB:C_GB + 24].rearrange("(m p) c -> p m c", p=128)), [k.w_in], [wg])
        S.dma("pool", I("dma_start", out=wg[:, :, 24:48], in_=k.w_in[l, :, C_SDT:C_SDT + 24].rearrange("(m p) c -> p m c", p=128)), [k.w_in], [wg])
        S.dma("sp", I("dma_start", out=gbias[:], in_=k.gate_bias[:, l, :]), [k.gate_bias], [gbias])
        S.dma("sp", I("dma_start", out=galog[:], in_=k.gate_alog[:, l, :]), [k.gate_alog], [galog])
        for half in range(2):
            ps = k.psrot.next()
            for i in range(8):
                n = half * 8 + i
                for kk in range(NK):
                    mm(S, ps[:, i * 48:(i + 1) * 48], k.hT[:, kk, n * 128:(n + 1) * 128], wg[:, kk, :], kk == 0, kk == NK - 1, [k.hT, wg], [ps])
            S.op("dve", I("tensor_tensor",
                out=GT[:, half * 8:(half + 1) * 8, :], in0=ps[:, 0:384].rearrange("p (n c) -> p n c", c=48),
                in1=gbias[:].unsqueeze(1).broadcast_to([128, 8, 48]), op=ALU.add), [ps, gbias], [GT])
        S.op("act", I("activation", out=k.beta[:], in_=GT[:, :, 0:12], func=AF.Sigmoid), [GT], [k.beta])
        S.op("act", I("activation", out=k.sp[:], in_=GT[:, :, 12:48], func=AF.Exp), [GT], [k.sp])
        S.op("act", I("activation", out=k.sp[:], in_=k.sp[:], func=AF.Ln, bias=k.onesF[:, 0:1], scale=1.0), [k.sp, k.onesF], [k.sp])
        S.op("act", I("activation", out=galog[:], in_=galog[:], func=AF.Exp), [galog], [galog])
        S.op("dve", I("scalar_tensor_tensor", out=k.gdec[:], in0=k.sp[:], scalar=-1.0, in1=galog[:, 12:48].unsqueeze(1).broadcast_to([128, NCK, 36]),
                                                     op0=ALU.mult, op1=ALU.mult), [k.sp, galog], [k.gdec])
        S.barrier()


def conv_chunk(k, T, l, ci, out_ap, out_buf, rot4):
    S = k.S
    w = T["w"].next()
    S.dma("pool", I("dma_start", out=w[:], in_=k.w_in[l, :, ci * 128:(ci + 1) * 128].rearrange("(m p) c -> p m c", p=128)), [k.w_in], [w])
    xp = T["xp"]
    acc = T["acc"]
    for t in range(4):
        ps = rot4.next()
        for kk in range(NK):
            mm(S, ps[:], w[:, kk, :], k.hT[:, kk, t * 512:(t + 1) * 512], kk == 0, kk == NK - 1, [w, k.hT], [ps])
        evac(S, t, xp[:, 2 + t * 512:2 + (t + 1) * 512], ps[:], [ps], [xp])
    S.op("dve", I("tensor_scalar", out=acc[:], in0=xp[:, 0:L], scalar1=k.cw[:, ci, 0:1], scalar2=None, op0=ALU.mult), [xp, k.cw], [acc])
    for j in range(1, 5):
        S.op("dve", I("scalar_tensor_tensor", out=acc[:], in0=xp[:, j:j + L], scalar=k.cw[:, ci, j:j + 1], in1=acc[:], op0=ALU.mult, op1=ALU.add), [xp, k.cw, acc], [acc])
    S.op("act", I("activation", out=out_ap, in_=acc[:], func=AF.Silu, bias=k.cb[:, ci:ci + 1], scale=1.0), [acc, k.cb], [out_buf])


def conv_temps(k, st, l):
    S = k.S
    T = {"w": Rot([S.sbuf(f"cvw{i}", [128, NK, 128], BF16, st) for i in range(2)]),
         "xp": S.sbuf("cvxp", [128, L + 4], F32, st), "acc": S.sbuf("cvacc", [128, L], F32, st)}
    S.op("dve", I("memset", T["xp"][:, 0:2], 0.0), [], [T["xp"]])
    S.op("dve", I("memset", T["xp"][:, L + 2:L + 4], 0.0), [], [T["xp"]])
    return T


def load_conv_params(k, l):
    S = k.S
    S.dma("sp", I("dma_start", out=k.cw[:], in_=k.conv_wT[:, l, :, :]), [k.conv_wT], [k.cw])
    S.dma("sp", I("dma_start", out=k.cb[:], in_=k.conv_bT[:, l, :]), [k.conv_bT], [k.cb])


def decay_prep(k, T, gview, d, rot4):
    S = k.S
    tri = k.mup if d == 0 else k.mlow
    g, gc, egc, egl, ekd, R, ET = T["g"], T["gc"], T["egc"], T["egl"], T["ekd"], T["R"], T["ET"]
    S.op("dve", I("tensor_copy", out=g[:], in_=gview), [k.gdec], [g])
    p1 = rot4.next()
    mm(S, p1[:, 0:NCK], tri[:], g[:], True, True, [tri, g], [p1])
    mm(S, p1[:, 64:64 + NCK], k.onesF[:], g[:], True, True, [k.onesF, g], [p1])
    S.op("dve", I("tensor_copy", out=gc[:], in_=p1[:, 0:NCK]), [p1], [gc])
    S.op("act", I("activation", out=egc[:], in_=p1[:, 0:NCK], func=AF.Exp), [p1], [egc])
    S.op("act", I("activation", out=egl[:], in_=p1[:, 64:64 + NCK], func=AF.Exp), [p1], [egl])
    S.op("dve", I("tensor_tensor", out=ekd[:], in0=p1[:, 64:64 + NCK], in1=gc[:], op=ALU.subtract), [p1, gc], [ekd])
    S.op("act", I("activation", out=ekd[:], in_=ekd[:], func=AF.Exp), [ekd], [ekd])
    S.op("pool", I("tensor_tensor", out=R[:].rearrange("p (n s) -> p n s", s=CH), in0=tri[:].unsqueeze(1).broadcast_to([128, NCK, CH]),
                                           in1=g[:].unsqueeze(2).broadcast_to([128, NCK, CH]), op=ALU.mult), [tri, g], [R])
    valid = k.mup if d == 0 else k.mlow
    for t in range(4):
        pb = rot4.next()
        mm(S, pb[:], k.onesF[:], R[:, t * 512:(t + 1) * 512], True, True, [k.onesF, R], [pb])
        S.op("dve", I("tensor_tensor",
            out=ET[:, t * 512:(t + 1) * 512].rearrange("p (n s) -> p n s", s=CH), in0=pb[:].rearrange("p (n s) -> p n s", s=CH),
            in1=gc[:, t * 4:(t + 1) * 4].unsqueeze(2).broadcast_to([128, 4, CH]), op=ALU.subtract), [pb, gc], [ET])
    S.op("dve", I("tensor_scalar", out=ET[:], in0=ET[:], scalar1=0.0, scalar2=None, op0=ALU.min), [ET], [ET])
    S.op("act", I("activation", out=ET[:], in_=ET[:], func=AF.Exp), [ET], [ET])
    S.op("pool", I("tensor_tensor", out=ET[:].rearrange("p (n s) -> p n s", s=CH), in0=ET[:].rearrange("p (n s) -> p n s", s=CH),
                                           in1=valid[:].unsqueeze(1).broadcast_to([128, NCK, CH]), op=ALU.mult), [ET, valid], [ET])
    for b in (ET, egc, egl, ekd):
        S.op("dve", I("tensor_scalar", out=b[:], in0=b[:], scalar1=1.0, scalar2=1.0, op0=ALU.add, op1=ALU.subtract), [b], [b])


def decay_temps(k, st):
    S = k.S
    T = {n: S.sbuf("dc" + n, [128, NCK], F32, st) for n in ("g", "gc", "egc", "egl", "ekd")}
    T["R"] = S.sbuf("dcR", [128, L], F32, st)
    T["ET"] = S.sbuf("dcET", [128, L], F32, st)
    return T


def mixer_ssd(k, l):
    S = k.S
    rot4 = Rot(k.PS[0:5])
    pA_r, pB_r, pC_r = k.PS[5], k.PS[6], k.PS[7]
    load_conv_params(k, l)
    with contextlib.ExitStack() as st0:
        ssq = S.sbuf("sdssq", [128, 2, NCK], F32, st0)
        dsk = S.sbuf("sddsk", [128, 12], F32, st0)
        nrm = S.sbuf("sdnrm", [128, 768], F32, st0)
        S.dma("sp", I("dma_start", out=dsk[:], in_=k.ssm_Dbc[:, l, :]), [k.ssm_Dbc], [dsk])
        S.dma("sp", I("dma_start", out=nrm[:], in_=k.ssm_normbc[:, l, :]), [k.ssm_normbc], [nrm])
        for g in range(2):
            with contextlib.ExitStack() as st:
                CT = S.sbuf("sdCT", [128, L], BF16, st)
                BT = S.sbuf("sdBT", [128, L], BF16, st)
                BTM = S.sbuf("sdBTM", [128, NCK, 128], BF16, st)
                xTM = S.sbuf("sdxTM", [128, NCK, 384], BF16, st)
                CBT = S.sbuf("sdCBT", [128, L], F32, st)
                yacc = S.sbuf("sdyacc", [128, NCK, 384], F32, st)
                with contextlib.ExitStack() as sta:
                    T = conv_temps(k, sta, l)
                    xfm = S.sbuf("sdxfm", [128, L], BF16, sta)
                    conv_chunk(k, T, l, 24 + g, BT[:], BT, rot4)
                    conv_chunk(k, T, l, 26 + g, CT[:], CT, rot4)
                    for n in range(NCK):
                        if n % 4 == 0:
                            pt = rot4.next()
                            ptb = pt[:].bitcast(BF16)
                        tr(S, ptb[:, (n % 4) * 128:(n % 4 + 1) * 128], BT[:, n * 128:(n + 1) * 128], k.identB[:], [BT, k.identB], [pt])
                        if n % 4 == 3:
                            evac(S, n // 4, BTM[:, n - 3:n + 1, :].rearrange("p a b -> p (a b)"), ptb[:, 0:512], [pt], [BTM])
                    for i in range(3):
                        conv_chunk(k, T, l, 18 + 3 * g + i, xfm[:], xfm, rot4)
                        for n in range(NCK):
                            if n % 4 == 0:
                                pt = rot4.next()
                                ptb = pt[:].bitcast(BF16)
                            tr(S, ptb[:, (n % 4) * 128:(n % 4 + 1) * 128], xfm[:, n * 128:(n + 1) * 128], k.identB[:], [xfm, k.identB], [pt])
                            if n % 4 == 3:
                                evac(S, n // 4, xTM[:, n - 3:n + 1, i * 128:(i + 1) * 128], ptb[:, 0:512].rearrange("p (a b) -> p a b", b=128), [pt], [xTM])
                    for n in range(NCK):
                        if n % 4 == 0:
                            pc = rot4.next()
                        mm(S, pc[:, (n % 4) * 128:(n % 4 + 1) * 128], BT[:, n * 128:(n + 1) * 128], CT[:, n * 128:(n + 1) * 128], True, True, [BT, CT], [pc])
                        if n % 4 == 3:
                            evac(S, n // 4, CBT[:, (n - 3) * 128:(n + 1) * 128], pc[:], [pc], [CBT])
                    S.barrier()
                for d in range(2):
                    with contextlib.ExitStack() as stb:
                        T = decay_temps(k, stb)
                        xdt = S.sbuf("sdxdt", [128, NCK, 384], BF16, stb)
                        xdts = S.sbuf("sdxdts", [128, NCK, 384], BF16, stb)
                        M = [S.sbuf(f"sdM{h}", [128, L], BF16, stb) for h in range(6)]
                        eac = S.sbuf("sdeac", [128, NCK, 6], F32, stb)
                        eal = S.sbuf("sdeal", [128, NCK, 6], F32, stb)
                        eks = S.sbuf("sdeks", [128, NCK, 6], F32, stb)
                        S32 = S.sbuf("sdS32", [128, 384], F32, stb)
                        Sbf = S.sbuf("sdSbf", [128, 384], BF16, stb)
                        tmr = Rot([S.sbuf(f"sdtm{i}", [128, 384], F32, stb) for i in range(2)])
                        tm2 = Rot([S.sbuf(f"sdtn{i}", [128, 384], F32, stb) for i in range(2)])
                        for h in range(6):
                            col = 12 + d * 12 + g * 6 + h
                            decay_prep(k, T, k.gdec[:, :, col], d, rot4)
                            S.op("dve", I("tensor_tensor", out=M[h][:], in0=CBT[:], in1=T["ET"][:], op=ALU.mult), [CBT, T["ET"]], [M[h]])
                            S.op("dve", I("tensor_copy", out=eac[:, :, h], in_=T["egc"][:]), [T["egc"]], [eac])
                            S.op("dve", I("tensor_copy", out=eal[:, :, h], in_=T["egl"][:]), [T["egl"]], [eal])
                            S.op("dve", I("tensor_copy", out=eks[:, :, h], in_=T["ekd"][:]), [T["ekd"]], [eks])
                        dtv = k.sp[:, :, 12 + d * 12 + g * 6:12 + d * 12 + g * 6 + 6]
                        S.op("dve", I("tensor_tensor", out=xdt[:].rearrange("p n (h q) -> p n h q", q=64), in0=xTM[:].rearrange("p n (h q) -> p n h q", q=64),
                                                              in1=dtv.unsqueeze(3).broadcast_to([128, NCK, 6, 64]), op=ALU.mult), [xTM, k.sp], [xdt])
                        S.op("dve", I("tensor_tensor", out=xdts[:].rearrange("p n (h q) -> p n h q", q=64), in0=xdt[:].rearrange("p n (h q) -> p n h q", q=64),
                                                              in1=eks[:].unsqueeze(3).broadcast_to([128, NCK, 6, 64]), op=ALU.mult), [xdt, eks], [xdts])
                        S.op("dve", I("memset", S32[:], 0.0), [], [S32])
                        S.op("dve", I("memset", Sbf[:], 0.0), [], [Sbf])
                        order = range(NCK) if d == 0 else range(NCK - 1, -1, -1)
                        for n in order:
                            mm(S, pA_r[:, 0:384], CT[:, n * 128:(n + 1) * 128], Sbf[:], True, True, [CT, Sbf], [pA_r])
                            for h in range(6):
                                mm(S, pB_r[:, h * 64:(h + 1) * 64], M[h][:, n * 128:(n + 1) * 128], xdt[:, n, h * 64:(h + 1) * 64], True, True, [M[h], xdt], [pB_r])
                            mm(S, pC_r[:, 0:384], BTM[:, n, :], xdts[:, n, :], True, True, [BTM, xdts], [pC_r])
                            tm = tmr.next()
                            S.op("dve", I("tensor_tensor", out=tm[:].rearrange("p (h q) -> p h q", q=64), in0=pA_r[:, 0:384].rearrange("p (h q) -> p h q", q=64),
                                                                               in1=eac[:, n, :].unsqueeze(2).broadcast_to([128, 6, 64]), op=ALU.mult), [pA_r, eac], [tm])
                            if d == 0:
                                S.op("dve", I("tensor_tensor", out=yacc[:, n, :], in0=pB_r[:, 0:384], in1=tm[:], op=ALU.add), [pB_r, tm], [yacc])
                            else:
                                t2 = tm2.next()
                                S.op("dve", I("tensor_tensor", out=t2[:], in0=pB_r[:, 0:384], in1=tm[:], op=ALU.add), [pB_r, tm], [t2])
                                S.op("pool", I("tensor_tensor", out=yacc[:, n, :], in0=yacc[:, n, :], in1=t2[:], op=ALU.add), [t2, yacc], [yacc])
                            S.op("pool", I("tensor_tensor", out=S32[:].rearrange("p (h q) -> p h q", q=64), in0=S32[:].rearrange("p (h q) -> p h q", q=64),
                                                                         in1=eal[:, n, :].unsqueeze(2).broadcast_to([128, 6, 64]), op=ALU.mult), [S32, eal], [S32])
                            S.op("dve", I("tensor_tensor", out=S32[:], in0=pC_r[:, 0:384], in1=S32[:], op=ALU.add), [pC_r, S32], [S32])
                            S.op("act", I("activation", out=Sbf[:], in_=S32[:], func=AF.Copy), [S32], [Sbf])
                        S.barrier()
                with contextlib.ExitStack() as stc:
                    wz = S.sbuf("sdwz", [128, NK, 384], BF16, stc)
                    szr = Rot([S.sbuf(f"sdsz{i}", [128, 384], F32, stc) for i in range(2)])
                    t1r = Rot([S.sbuf(f"sdt1{i}", [128, 384], F32, stc) for i in range(2)])
                    S.dma("pool", I("dma_start", out=wz[:], in_=k.w_in[l, :, C_SZ + g * 384:C_SZ + (g + 1) * 384].rearrange("(m p) c -> p m c", p=128)), [k.w_in], [wz])
                    for n in range(NCK):
                        pz = rot4.next()
                        for kk in range(NK):
                            mm(S, pz[:, 0:384], k.hT[:, kk, n * 128:(n + 1) * 128], wz[:, kk, :], kk == 0, kk == NK - 1, [k.hT, wz], [pz])
                        sz = szr.next()
                        t1 = t1r.next()
                        S.op("act", I("activation", out=sz[:], in_=pz[:, 0:384], func=AF.Silu), [pz], [sz])
                        S.op("dve", I("tensor_tensor", out=t1[:].rearrange("p (h q) -> p h q", q=64), in0=xTM[:, n, :].rearrange("p (h q) -> p h q", q=64),
                                                                           in1=dsk[:, g * 6:(g + 1) * 6].unsqueeze(2).broadcast_to([128, 6, 64]), op=ALU.mult), [xTM, dsk], [t1])
                        S.op("dve", I("tensor_tensor", out=t1[:], in0=t1[:], in1=yacc[:, n, :], op=ALU.add), [t1, yacc], [t1])
                        S.op("pool", I("tensor_tensor", out=yacc[:, n, :], in0=t1[:], in1=sz[:], op=ALU.mult), [t1, sz, yacc], [yacc])
                        S.op("act", I("activation", out=t1[:], in_=yacc[:, n, :], func=AF.Square, accum_out=ssq[:, g, n:n + 1]), [yacc], [t1, ssq])
                    S.dma("sp", I("dma_start", out=k.yss[g], in_=yacc[:].rearrange("p a b -> p (a b)")), [yacc], [k.yss])
                    S.barrier()
        with contextlib.ExitStack() as st:
            rstd = S.sbuf("sdrstd", [128, NCK], F32, st)
            yz = S.sbuf("sdyz", [128, NCK, 384], F32, st)
            ybr = Rot([S.sbuf(f"sdyb{i}", [128, 384], BF16, st) for i in range(2)])
            yT = S.sbuf("sdyT", [128, 3, L], BF16, st)
            S.op("dve", I("tensor_tensor", out=rstd[:], in0=ssq[:, 0, :], in1=ssq[:, 1, :], op=ALU.add), [ssq], [rstd])
            S.op("act", I("activation", out=rstd[:], in_=rstd[:], func=AF.Sqrt, bias=k.epsT[:], scale=1.0 / 768), [rstd, k.epsT], [rstd])
            S.op("dve", I("reciprocal", out=rstd[:], in_=rstd[:]), [rstd], [rstd])
            for g in range(2):
                S.dma("sp", I("dma_start", out=yz[:].rearrange("p a b -> p (a b)"), in_=k.yss[g]), [k.yss], [yz])
                for n in range(NCK):
                    yb = ybr.next()
                    S.op("dve", I("scalar_tensor_tensor", out=yb[:], in0=yz[:, n, :], scalar=rstd[:, n:n + 1], in1=nrm[:, g * 384:(g + 1) * 384],
                                                                               op0=ALU.mult, op1=ALU.mult), [yz, rstd, nrm], [yb])
                    pt = rot4.next()
                    ptb = pt[:].bitcast(BF16)
                    for i in range(3):
                        tr(S, ptb[:, i * 128:(i + 1) * 128], yb[:, i * 128:(i + 1) * 128], k.identB[:], [yb, k.identB], [pt])
                    evac(S, n, yT[:, :, n * 128:(n + 1) * 128], ptb[:, 0:384].rearrange("p (a b) -> p a b", b=128), [pt], [yT])
                for i in range(3):
                    r0 = 768 + g * 384 + i * 128
                    S.dma("sp", I("dma_start", out=k.mixT[r0:r0 + 128, :], in_=yT[:, i, :]), [yT], [k.mixT])
            S.barrier()


def mixer_gdn(k, l):
    S = k.S
    rot4 = Rot(k.PS[0:4])
    pks, pv, po, pS = k.PS[4], k.PS[5], k.PS[6], k.PS[7]
    load_conv_params(k, l)
    with contextlib.ExitStack() as st0:
        gn = S.sbuf("gdgn", [128, DEPTH], F32, st0)
        k.lvl = S.sbuf("gdlvl", [128, 7, 128], F32, st0)
        S.dma("sp", I("dma_start", out=k.lvl[:], in_=k.c_lvl[:]), [k.c_lvl], [k.lvl])
        S.dma("sp", I("dma_start", out=gn[:], in_=k.gdn_normT[:]), [k.gdn_normT], [gn])
        for h in range(6):
            with contextlib.ExitStack() as st:
                qT = S.sbuf("gdq", [128, L], BF16, st)
                kT = S.sbuf("gdk", [128, L], BF16, st)
                kTM = S.sbuf("gdkTM", [128, NCK, 128], BF16, st)
                kTf = S.sbuf("gdkTf", [128, L], F32, st)
                vTM = S.sbuf("gdvTM", [128, NCK, 128], BF16, st)
                zs = S.sbuf("gdzs", [128, L], F32, st)
                oacc = S.sbuf("gdo", [128, L], F32, st)
                with contextlib.ExitStack() as sta:
                    T = conv_temps(k, sta, l)
                    qf = S.sbuf("gdqf", [128, L], F32, sta)
                    vf = S.sbuf("gdvf", [128, L], BF16, sta)
                    sqr = Rot([S.sbuf(f"gdsq{i}", [128, 512], F32, sta) for i in range(2)])
                    rr = Rot([S.sbuf(f"gdrr{i}", [128, 512], F32, sta) for i in range(2)])
                    for ci, dst, scl in ((h, qT, 128 ** -0.5), (6 + h, kT, 1.0)):
                        conv_chunk(k, T, l, ci, qf[:], qf, rot4)
                        for t in range(4):
                            sq = sqr.next()
                            r = rr.next()
                            S.op("act", I("activation", out=sq[:], in_=qf[:, t * 512:(t + 1) * 512], func=AF.Square), [qf], [sq])
                            ps = rot4.next()
                            mm(S, ps[:], k.onesF[:], sq[:], True, True, [k.onesF, sq], [ps])
                            S.op("act", I("activation", out=r[:], in_=ps[:], func=AF.Sqrt, bias=k.epsT[:], scale=1.0), [ps, k.epsT], [r])
                            S.op("dve", I("reciprocal", out=r[:], in_=r[:]), [r], [r])
                            S.op("dve", I("scalar_tensor_tensor", out=dst[:, t * 512:(t + 1) * 512], in0=qf[:, t * 512:(t + 1) * 512], scalar=scl, in1=r[:],
                                          op0=ALU.mult, op1=ALU.mult), [qf, r], [dst])
                            if dst is kT:
                                S.op("pool", I("tensor_tensor", out=kTf[:, t * 512:(t + 1) * 512], in0=qf[:, t * 512:(t + 1) * 512], in1=r[:], op=ALU.mult), [qf, r], [kTf])
                    conv_chunk(k, T, l, 12 + h, vf[:], vf, rot4)
                    for src, dstm in ((vf, vTM), (kT, kTM)):
                        for n in range(NCK):
                            if n % 4 == 0:
                                pt = rot4.next()
                                ptb = pt[:].bitcast(BF16)
                            tr(S, ptb[:, (n % 4) * 128:(n % 4 + 1) * 128], src[:, n * 128:(n + 1) * 128], k.identB[:], [src, k.identB], [pt])
                            if n % 4 == 3:
                                evac(S, n // 4, dstm[:, n - 3:n + 1, :].rearrange("p a b -> p (a b)"), ptb[:, 0:512], [pt], [dstm])
                    w = T["w"].next()
                    S.dma("pool", I("dma_start", out=w[:], in_=k.w_in[l, :, C_GZ + h * 128:C_GZ + (h + 1) * 128].rearrange("(m p) c -> p m c", p=128)), [k.w_in], [w])
                    for t in range(4):
                        ps = rot4.next()
                        for kk in range(NK):
                            mm(S, ps[:], w[:, kk, :], k.hT[:, kk, t * 512:(t + 1) * 512], kk == 0, kk == NK - 1, [w, k.hT], [ps])
                        S.op("act", I("activation", out=zs[:, t * 512:(t + 1) * 512], in_=ps[:], func=AF.Silu), [ps], [zs])
                    S.barrier()
                for d in range(2):
                    if k.gstage < 2:
                        break
                    with contextlib.ExitStack() as stb:
                        T = decay_temps(k, stb)
                        QS = [S.sbuf(f"gdQS{i}", [128, 512], BF16, stb) for i in range(4)]
                        QC = [S.sbuf(f"gdQC{i}", [128, 512], BF16, stb) for i in range(4)]
                        TT = [S.sbuf(f"gdTT{i}", [128, 512], BF16, stb) for i in range(4)]
                        TC = [S.sbuf(f"gdTC{i}", [128, 512], BF16, stb) for i in range(4)]
                        qmr = Rot([S.sbuf(f"gdqm{i}", [128, 512], BF16, stb) for i in range(2)])
                        zr = Rot([S.sbuf(f"gdz{i}", [128, 512], BF16, stb) for i in range(2)])
                        qkm = S.sbuf("gdqkm", [128, L], BF16, stb)
                        qd = S.sbuf("gdqd", [128, L], BF16, stb)
                        bt = S.sbuf("gdbt", [128, NCK], F32, stb)
                        nbt = S.sbuf("gdnbt", [128, NCK], F32, stb)
                        negegc = S.sbuf("gdneg", [128, NCK], F32, stb)
                        bekd = S.sbuf("gdbekd", [128, NCK], F32, stb)
                        nstrict = S.sbuf("gdnst", [128, 128], F32, stb)
                        S32 = S.sbuf("gdS32", [128, 128], F32, stb)
                        Sbf = S.sbuf("gdSbf", [128, 128], BF16, stb)
                        tmp = Rot([S.sbuf(f"gdtmp{i}", [128, 512], F32, stb) for i in range(2)])
                        Ur = Rot([S.sbuf(f"gdU{i}", [128, 128], BF16, stb) for i in range(2)])
                        vnr = Rot([S.sbuf(f"gdvn{i}", [128, 128], BF16, stb) for i in range(2)])
                        vsr = Rot([S.sbuf(f"gdvs{i}", [128, 128], BF16, stb) for i in range(2)])
                        col = d * 6 + h
                        decay_prep(k, T, k.gdec[:, :, col], d, rot4)
                        ET = T["ET"]
                        valid = k.mup if d == 0 else k.mlow
                        S.op("dve", I("tensor_copy", out=bt[:], in_=k.beta[:, :, col]), [k.beta], [bt])
                        S.op("dve", I("tensor_scalar", out=nbt[:], in0=bt[:], scalar1=-1.0, scalar2=None, op0=ALU.mult), [bt], [nbt])
                        S.op("dve", I("tensor_scalar", out=negegc[:], in0=T["egc"][:], scalar1=-1.0, scalar2=None, op0=ALU.mult), [T["egc"]], [negegc])
                        S.op("dve", I("tensor_tensor", out=bekd[:], in0=bt[:], in1=T["ekd"][:], op=ALU.mult), [bt, T["ekd"]], [bekd])
                        S.op("dve", I("tensor_tensor", out=nstrict[:], in0=valid[:], in1=k.identF[:], op=ALU.subtract), [valid, k.identF], [nstrict])
                        S.op("pool", I("tensor_tensor", out=T["R"][:].rearrange("p (n s) -> p n s", s=CH), in0=k.identF[:].unsqueeze(1).broadcast_to([128, NCK, CH]),
                                       in1=T["egc"][:].unsqueeze(2).broadcast_to([128, NCK, CH]), op=ALU.mult), [k.identF, T["egc"]], [T["R"]])
                        for t in range(4):
                            pb = rot4.next()
                            mm(S, pb[:], k.onesF[:], T["R"][:, t * 512:(t + 1) * 512], True, True, [k.onesF, T["R"]], [pb])
                            S.op("dve", I("tensor_tensor", out=qd[:, t * 512:(t + 1) * 512], in0=pb[:], in1=qT[:, t * 512:(t + 1) * 512], op=ALU.mult), [pb, qT], [qd])
                            S.op("pool", I("tensor_scalar", out=qd[:, t * 512:(t + 1) * 512], in0=qd[:, t * 512:(t + 1) * 512], scalar1=1.0, scalar2=1.0, op0=ALU.add, op1=ALU.subtract), [qd], [qd])
                        for t in range(4):
                            pk = rot4.next()
                            pq = rot4.next()
                            for i in range(4):
                                n = t * 4 + i
                                mm(S, pk[:, i * 128:(i + 1) * 128], kT[:, n * 128:(n + 1) * 128], kT[:, n * 128:(n + 1) * 128], True, True, [kT], [pk])
                                mm(S, pq[:, i * 128:(i + 1) * 128], kT[:, n * 128:(n + 1) * 128], qT[:, n * 128:(n + 1) * 128], True, True, [kT, qT], [pq])
                            tm = tmp.next()
                            S.op("dve", I("tensor_tensor", out=tm[:], in0=pk[:], in1=ET[:, t * 512:(t + 1) * 512], op=ALU.mult), [pk, ET], [tm])
                            S.op("pool", I("tensor_tensor", out=tm[:].rearrange("p (n s) -> p n s", s=CH), in0=tm[:].rearrange("p (n s) -> p n s", s=CH),
                                           in1=nstrict[:].unsqueeze(1).broadcast_to([128, 4, CH]), op=ALU.mult), [tm, nstrict], [tm])
                            S.op("dve", I("tensor_tensor", out=QS[t][:].rearrange("p (n s) -> p n s", s=CH), in0=tm[:].rearrange("p (n s) -> p n s", s=CH),
                                          in1=nbt[:, t * 4:(t + 1) * 4].unsqueeze(2).broadcast_to([128, 4, CH]), op=ALU.mult), [tm, nbt], [QS[t]])
                            S.op("dve", I("tensor_tensor", out=qkm[:, t * 512:(t + 1) * 512], in0=pq[:], in1=ET[:, t * 512:(t + 1) * 512], op=ALU.mult), [pq, ET], [qkm])
                            pt = rot4.next()
                            ptb = pt[:].bitcast(BF16)
                            for i in range(4):
                                tr(S, ptb[:, i * 128:(i + 1) * 128], QS[t][:, i * 128:(i + 1) * 128], k.identB[:], [QS[t], k.identB], [pt])
                            S.op("act", I("activation", out=QC[t][:], in_=ptb[:, 0:512], func=AF.Copy), [pt], [QC[t]])
                        for t in range(4):
                            for X in (TT[t], TC[t]):
                                S.op("pool", I("tensor_copy", out=X[:].rearrange("p (n s) -> p n s", s=CH),
                                               in_=k.identB[:].unsqueeze(1).broadcast_to([128, 4, CH])), [k.identB], [X])
                        for lev in range(7 if k.gstage >= 3 else 0):
                            for t in range(4):
                                qm = qmr.next()
                                S.op("pool", I("tensor_tensor", out=qm[:].rearrange("p (n s) -> p n s", s=CH), in0=QC[t][:].rearrange("p (n s) -> p n s", s=CH),
                                               in1=k.lvl[:, lev, :].unsqueeze(1).broadcast_to([128, 4, CH]), op=ALU.mult), [QC[t], k.lvl], [qm])
                                pz = rot4.next()
                                for i in range(4):
                                    sl = slice(i * 128, (i + 1) * 128)
                                    mm(S, pz[:, sl], qm[:, sl], TT[t][:, sl], True, True, [qm, TT[t]], [pz])
                                z = zr.next()
                                S.op("dve", I("tensor_scalar", out=z[:], in0=pz[:], scalar1=1.0, scalar2=1.0, op0=ALU.add, op1=ALU.subtract), [pz], [z])
                                pw = rot4.next()
                                pwt = rot4.next()
                                for i in range(4):
                                    sl = slice(i * 128, (i + 1) * 128)
                                    mm(S, pw[:, sl], TC[t][:, sl], z[:, sl], True, True, [TC[t], z], [pw])
                                    mm(S, pwt[:, sl], z[:, sl], TC[t][:, sl], True, True, [TC[t], z], [pwt])
                                S.op("dve", I("tensor_tensor", out=TT[t][:], in0=pw[:], in1=TT[t][:], op=ALU.add), [pw, TT[t]], [TT[t]])
                                S.op("pool", I("tensor_scalar", out=TT[t][:], in0=TT[t][:], scalar1=1.0, scalar2=1.0, op0=ALU.add, op1=ALU.subtract), [TT[t]], [TT[t]])
                                S.op("dve", I("tensor_tensor", out=TC[t][:], in0=pwt[:], in1=TC[t][:], op=ALU.add), [pwt, TC[t]], [TC[t]])
                                S.op("pool", I("tensor_scalar", out=TC[t][:], in0=TC[t][:], scalar1=1.0, scalar2=1.0, op0=ALU.add, op1=ALU.subtract), [TC[t]], [TC[t]])
                        S.op("dve", I("memset", S32[:], 0.0), [], [S32])
                        S.op("dve", I("memset", Sbf[:], 0.0), [], [Sbf])
                        order = list(range(NCK)) if d == 0 else list(range(NCK - 1, -1, -1))
                        for idx, n in enumerate(order if k.gstage >= 4 else []):
                            cs = slice(n * 128, (n + 1) * 128)
                            t, i = n // 4, n % 4
                            mm(S, pks[:, 0:128], kTf[:, cs], S32[:], True, True, [kTf, S32], [pks])
                            U = Ur.next()
                            S.op("dve", I("scalar_tensor_tensor", out=U[:], in0=pks[:, 0:128], scalar=negegc[:, n:n + 1], in1=vTM[:, n, :], op0=ALU.mult, op1=ALU.add), [pks, negegc, vTM], [U])
                            S.op("pool", I("tensor_scalar", out=U[:], in0=U[:], scalar1=1.0, scalar2=1.0, op0=ALU.add, op1=ALU.subtract), [U], [U])
                            mm(S, pv[:, 0:128], TT[t][:, i * 128:(i + 1) * 128], U[:], True, True, [TT[t], U], [pv])
                            vn = vnr.next()
                            vs = vsr.next()
                            S.op("dve", I("tensor_scalar", out=vn[:], in0=pv[:, 0:128], scalar1=bt[:, n:n + 1], scalar2=None, op0=ALU.mult), [pv, bt], [vn])
                            S.op("dve", I("tensor_scalar", out=vs[:], in0=pv[:, 0:128], scalar1=bekd[:, n:n + 1], scalar2=None, op0=ALU.mult), [pv, bekd], [vs])
                            S.op("pool", I("tensor_scalar", out=vn[:], in0=vn[:], scalar1=1.0, scalar2=1.0, op0=ALU.add, op1=ALU.subtract), [vn], [vn])
                            S.op("pool", I("tensor_scalar", out=vs[:], in0=vs[:], scalar1=1.0, scalar2=1.0, op0=ALU.add, op1=ALU.subtract), [vs], [vs])
                            osl = slice(i * 128, (i + 1) * 128)
                            mm(S, po[:, osl], Sbf[:], qd[:, cs], True, False, [Sbf, qd], [po])
                            mm(S, po[:, osl], vn[:], qkm[:, cs], False, True, [vn, qkm], [po])
                            mm(S, pS[:, 0:128], kTM[:, n, :], vs[:], True, True, [kTM, vs], [pS])
                            S.op("dve", I("scalar_tensor_tensor", out=S32[:], in0=S32[:], scalar=T["egl"][:, n:n + 1], in1=pS[:, 0:128], op0=ALU.mult, op1=ALU.add), [S32, T["egl"], pS], [S32])
                            S.op("pool", I("tensor_scalar", out=S32[:], in0=S32[:], scalar1=1.0, scalar2=1.0, op0=ALU.add, op1=ALU.subtract), [S32], [S32])
                            S.op("act", I("activation", out=Sbf[:], in_=S32[:], func=AF.Copy), [S32], [Sbf])
                            if idx % 4 == 3:
                                ts_ = slice(t * 512, (t + 1) * 512)
                                if d == 0:
                                    S.op("act", I("activation", out=oacc[:, ts_], in_=po[:], func=AF.Copy), [po], [oacc])
                                else:
                                    S.op("dve", I("tensor_tensor", out=oacc[:, ts_], in0=po[:], in1=oacc[:, ts_], op=ALU.add), [po, oacc], [oacc])
                        S.barrier()
                with contextlib.ExitStack() as stc:
                    sqr = Rot([S.sbuf(f"gdsq2{i}", [128, 512], F32, stc) for i in range(2)])
                    rr = Rot([S.sbuf(f"gdrr2{i}", [128, 512], F32, stc) for i in range(2)])
                    yb = S.sbuf("gdyb", [128, L], BF16, stc)
                    for t in range(4):
                        ts_ = slice(t * 512, (t + 1) * 512)
                        sq = sqr.next()
                        r = rr.next()
                        S.op("act", I("activation", out=sq[:], in_=oacc[:, ts_], func=AF.Square), [oacc], [sq])
                        ps = rot4.next()
                        mm(S, ps[:], k.onesF[:], sq[:], True, True, [k.onesF, sq], [ps])
                        S.op("act", I("activation", out=r[:], in_=ps[:], func=AF.Sqrt, bias=k.epsT[:], scale=1.0 / 128), [ps, k.epsT], [r])
                        S.op("dve", I("reciprocal", out=r[:], in_=r[:]), [r], [r])
                        S.op("dve", I("tensor_tensor", out=r[:], in0=r[:], in1=oacc[:, ts_], op=ALU.mult), [r, oacc], [r])
                        S.op("dve", I("scalar_tensor_tensor", out=yb[:, ts_], in0=r[:], scalar=gn[:, l:l + 1], in1=zs[:, ts_], op0=ALU.mult, op1=ALU.mult), [r, gn, zs], [yb])
                    S.dma("sp", I("dma_start", out=k.mixT[h * 128:(h + 1) * 128, :], in_=yb[:]), [yb], [k.mixT])
                    S.barrier()


_CACHE = {}


def _consts():
    p = np.arange(128)
    c = {}
    c["c_identF"] = np.eye(128, dtype=np.float32)
    c["c_mlow"] = (p[:, None] >= p[None, :]).astype(np.float32)
    c["c_mup"] = (p[None, :] >= p[:, None]).astype(np.float32)
    cols = np.arange(64)
    cs = np.clip(cols - 8, 0, 48)
    inwin = (cols[None, :] >= cs[:, None]) & (cols[None, :] < cs[:, None] + 16)
    c["c_namask"] = np.where(inwin, 0.0, -30000.0).astype(np.float32)
    sel = np.zeros((8, 8, 128), np.float32)
    for e in range(8):
        sel[e, e, :] = 1.0
    c["c_sel"] = sel
    lv = np.zeros((128, 7, 128), np.float32)
    for i in range(7):
        bsz = 1 << i
        lv[:, i, :] = ((p[:, None] // (2 * bsz) == p[None, :] // (2 * bsz)) & (p[:, None] // bsz != p[None, :] // bsz))
    c["c_lvl"] = lv
    return c


def _layout(inp, b):
    f = np.float32
    g = {}
    g["x"] = np.ascontiguousarray(inp["x"][b])
    g["cT"] = np.ascontiguousarray(inp["c"][b].reshape(NK, 128).T)
    g["ada_bT"] = np.ascontiguousarray(inp["ada_b"].reshape(DEPTH, 96, 128).transpose(2, 0, 1))
    g["nmixT"] = np.ascontiguousarray(inp["norm_mix"].reshape(DEPTH, NK, 128).transpose(2, 0, 1))
    g["nffnT"] = np.ascontiguousarray(inp["norm_ffn"].reshape(DEPTH, NK, 128).transpose(2, 0, 1))
    g["nfinT"] = np.ascontiguousarray(inp["norm_final"].reshape(NK, 128).T)
    g["conv_wT"] = np.ascontiguousarray(inp["conv_w"].reshape(DEPTH, 5, 28, 128).transpose(3, 0, 2, 1))
    g["conv_bT"] = np.ascontiguousarray(inp["conv_b"].reshape(DEPTH, 28, 128).transpose(2, 0, 1))
    gb = np.zeros((DEPTH, 48), f)
    gb[:, 12:24] = inp["gdn_dt_bias"].reshape(DEPTH, 12)
    gb[:, 24:48] = inp["ssm_dt_bias"].reshape(DEPTH, 24)
    ga = np.zeros((DEPTH, 48), f)
    ga[:, 12:24] = inp["gdn_A_log"].reshape(DEPTH, 12)
    ga[:, 24:48] = inp["ssm_A_log"].reshape(DEPTH, 24)
    g["gate_bias"] = np.ascontiguousarray(np.broadcast_to(gb[None], (128, DEPTH, 48)))
    g["gate_alog"] = np.ascontiguousarray(np.broadcast_to(ga[None], (128, DEPTH, 48)))
    g["gdn_normT"] = np.ascontiguousarray(inp["gdn_norm"].T)
    g["ssm_Dbc"] = np.ascontiguousarray(np.broadcast_to(inp["ssm_D"][None], (128, DEPTH, 12)))
    g["ssm_normbc"] = np.ascontiguousarray(np.broadcast_to(inp["ssm_norm"][None], (128, DEPTH, 768)))
    cols = np.arange(64)
    dc = np.clip(cols[None, :] - cols[:, None], -15, 15) + 15
    rpb = inp["na_rpb"]
    g["na_rb"] = np.ascontiguousarray(rpb[:, :, :, dc].transpose(0, 1, 3, 2, 4).reshape(DEPTH, 8, 64, 15 * 64))
    for name in ("ada_w", "w_in", "w_out", "ffn_w1", "ffn_w3", "ffn_w2", "moe_router", "moe_w1", "moe_w3", "moe_w2"):
        g[name] = inp[name]
    g.update(_consts())
    return g


def kernel(**inputs):
    inp = {n: np.asarray(v, dtype=np.float32) for n, v in inputs.items()}
    if "nc" not in _CACHE:
        _CACHE["nc"] = build()
    nc = _CACHE["nc"]
    shared = None
    in_maps = []
    for b in range(8):
        m = _layout(inp, b) if shared is None else dict(shared)
        if shared is None:
            shared = m
        else:
            m["x"] = np.ascontiguousarray(inp["x"][b])
            m["cT"] = np.ascontiguousarray(inp["c"][b].reshape(NK, 128).T)
        in_maps.append(m)
    res = run_bass_kernel_spmd(nc, in_maps, core_ids=list(range(8)))
    return np.stack([res.results[b]["out"] for b in range(8)], axis=0).astype(np.float32)
```

```python
import contextlib
import numpy as np
import concourse.bass as bass
import concourse.mybir as mybir
from concourse.bass_utils import run_bass_kernel_spmd

F32 = mybir.dt.float32
BF16 = mybir.dt.bfloat16
ALU = mybir.AluOpType
AF = mybir.ActivationFunctionType
AX = mybir.AxisListType

D = 2048
L = 2048
DEPTH = 4
NK = 16
IN_W = 6704
CONV_CH = 3584
DFF = 5632
NF = 44
NE = 8
EPS = 1e-6
CH = 128
NCK = L // CH
C_GQ, C_GK, C_GV, C_SX, C_SB, C_SC = 0, 768, 1536, 2304, 3072, 3328
C_GZ, C_GB, C_GA, C_SZ, C_SDT, C_NQ, C_NK, C_NV = 3584, 4352, 4364, 4376, 5144, 5168, 5680, 6192

N_DMA_SEMS = 6


class Buf:
    __slots__ = ("t", "w", "r", "name")

    def __init__(self, t, name=""):
        self.t = t
        self.w = {}
        self.r = {}
        self.name = name

    def __getitem__(self, idx):
        return self.t[idx]


class Sched:
    ENGS = ("pe", "act", "dve", "pool", "sp")

    def __init__(self, nc, stack, same_engine_sync=True):
        self.nc = nc
        self.stack = stack
        self.same = same_engine_sync
        self.sems = {}
        self.cnt = {}
        for e in self.ENGS:
            self._mksem(e)
        self.dq = {}
        for q in ("sp", "pool", "act"):
            keys = []
            for i in range(N_DMA_SEMS):
                k = f"d_{q}{i}"
                self._mksem(k)
                keys.append(k)
            self.dq[q] = [keys, 0]
        self.ops = {e: [] for e in self.ENGS}
        self.seen = {e: {} for e in self.ENGS}

    def _mksem(self, key):
        self.sems[key] = self.stack.enter_context(self.nc.semaphore(key))
        self.cnt[key] = 0

    def sbuf(self, name, shape, dt, st=None):
        self.uid = getattr(self, "uid", 0) + 1
        name = f"{name}_{self.uid}"
        t = (st or self.stack).enter_context(self.nc.sbuf_tensor(name, list(shape), dt))
        return Buf(t, name)

    def psum(self, name, shape, dt=F32):
        t = self.stack.enter_context(self.nc.psum_tensor(name, list(shape), dt))
        return Buf(t, name)

    def _deps(self, reads, writes):
        deps = {}
        for b in reads:
            for k, v in b.w.items():
                if deps.get(k, 0) < v:
                    deps[k] = v
        for b in writes:
            for d in (b.w, b.r):
                for k, v in d.items():
                    if deps.get(k, 0) < v:
                        deps[k] = v
        return deps

    def _emit_waits(self, eng, deps):
        seen = self.seen[eng]
        for k, v in deps.items():
            if k == eng and (not self.same or eng == "pe"):
                continue
            if seen.get(k, 0) >= v:
                continue
            seen[k] = v
            self.ops[eng].append(("wait", k, v))

    def op(self, eng, fn, reads=(), writes=()):
        deps = self._deps(reads, writes)
        self._emit_waits(eng, deps)
        self.cnt[eng] += 1
        v = self.cnt[eng]
        self.ops[eng].append(("op", fn, eng, 1))
        for b in reads:
            b.r[eng] = v
        for b in writes:
            b.w = {eng: v}
            b.r = {}

    def dma(self, q, fn, reads=(), writes=()):
        keys, idx = self.dq[q]
        k = keys[idx % len(keys)]
        self.dq[q][1] = idx + 1
        deps = self._deps(reads, writes)
        if self.cnt[k] > 0:
            deps[k] = max(deps.get(k, 0), self.cnt[k])
        self._emit_waits(q, deps)
        self.cnt[k] += 16
        v = self.cnt[k]
        self.ops[q].append(("op", fn, k, 16))
        for b in reads:
            b.r[k] = v
        for b in writes:
            b.w = {k: v}
            b.r = {}

    def barrier(self):
        for e in self.ENGS:
            deps = {k: v for k, v in self.cnt.items() if v > 0 and k != e}
            if self.same and e != "pe" and self.cnt[e] > 0:
                deps[e] = self.cnt[e]
            self._emit_waits(e, deps)

    def finish(self):
        self.barrier()
        nc = self.nc
        with nc.Block() as block:
            def mk(ename):
                def body(eng):
                    for item in self.ops[ename]:
                        if item[0] == "wait":
                            eng.wait_ge(self.sems[item[1]], item[2])
                        else:
                            _, fn, k, inc = item
                            fn(eng).then_inc(self.sems[k], inc)
                return body
            block.tensor(mk("pe"))
            block.scalar(mk("act"))
            block.vector(mk("dve"))
            block.gpsimd(mk("pool"))
            block.sync(mk("sp"))


def I(method, *a, **kw):
    return lambda e: getattr(e, method)(*a, **kw)


class Rot:
    def __init__(self, bufs):
        self.b = bufs
        self.i = 0

    def next(self):
        b = self.b[self.i % len(self.b)]
        self.i += 1
        return b


LAZY = {
    "ada_w": [DEPTH, D, 6 * D], "w_in": [DEPTH, D, IN_W], "w_out": [DEPTH, D, D],
    "ffn_w1": [2, D, DFF], "ffn_w3": [2, D, DFF], "ffn_w2": [2, DFF, D],
    "moe_router": [2, D, NE], "moe_w1": [2, NE, D, DFF], "moe_w3": [2, NE, D, DFF], "moe_w2": [2, NE, DFF, D],
    "na_rb": [DEPTH, 8, 64, 15 * 64],
}


class K:
    def __getattr__(self, name):
        if name in LAZY:
            b = Buf(self.nc.dram_tensor(name, list(LAZY[name]), F32, kind="ExternalInput").ap(), name)
            setattr(self, name, b)
            self.used.append(name)
            return b
        raise AttributeError(name)


def build(nlayers=DEPTH, flags="gsnf", dbg=()):
    nc = bass.Bass("TRN2", target_bir_lowering=False)
    k = K()
    k.nc = nc
    k.used = []
    k.flags = flags
    k.gstage = min([int(c) for c in flags if c.isdigit()] or [9])
    k.dbg = dbg

    def din(name, shape, dt=F32):
        return Buf(nc.dram_tensor(name, list(shape), dt, kind="ExternalInput").ap(), name)

    def dscr(name, shape, dt, out=False):
        kind = "ExternalOutput" if (out or name in dbg) else "Internal"
        return Buf(nc.dram_tensor(name, list(shape), dt, kind=kind).ap(), name)

    k.x = din("x", [L, D])
    k.cT = din("cT", [128, NK])
    k.ada_bT = din("ada_bT", [128, DEPTH, 96])
    k.nmixT = din("nmixT", [128, DEPTH, NK])
    k.nffnT = din("nffnT", [128, DEPTH, NK])
    k.nfinT = din("nfinT", [128, NK])
    k.conv_wT = din("conv_wT", [128, DEPTH, 28, 5])
    k.conv_bT = din("conv_bT", [128, DEPTH, 28])
    k.gate_bias = din("gate_bias", [128, DEPTH, 48])
    k.gate_alog = din("gate_alog", [128, DEPTH, 48])
    k.gdn_normT = din("gdn_normT", [128, DEPTH])
    k.ssm_Dbc = din("ssm_Dbc", [128, DEPTH, 12])
    k.ssm_normbc = din("ssm_normbc", [128, DEPTH, 768])
    k.c_identF = din("c_identF", [128, 128])
    k.c_mlow = din("c_mlow", [128, 128])
    k.c_mup = din("c_mup", [128, 128])
    k.c_namask = din("c_namask", [64, 64])
    k.c_sel = din("c_sel", [8, 8, 128])
    k.c_lvl = din("c_lvl", [128, 7, 128])
    k.out = dscr("out", [L, D], F32, out=True)
    k.xT = dscr("xT", [NK, 128, L], F32)
    k.mixT = dscr("mixT", [D, L], BF16)
    k.h2T = dscr("h2T", [NK, 128, L], BF16)
    k.yss = dscr("yss", [2, 128, NCK * 384], F32)
    if "modT" in dbg:
        k.dmod = dscr("modT", [128, DEPTH * 96], F32)
        k.dacc = dscr("accdbg", [128, NK * 1024], F32, out=True)
        k.dh2 = dscr("h2dump", [128, NK * L], BF16, out=True)

    with contextlib.ExitStack() as st:
        S = Sched(nc, st)
        k.S = S
        k.PS = [S.psum(f"ps{i}", [128, 512], F32) for i in range(8)]
        k.psrot = Rot(k.PS)
        k.identF = S.sbuf("identF", [128, 128], F32)
        k.identB = S.sbuf("identB", [128, 128], BF16)
        k.onesF = S.sbuf("onesF", [128, 128], F32)
        k.zerosF = S.sbuf("zerosF", [128, 128], F32)
        k.mlow = S.sbuf("mlow", [128, 128], F32)
        k.mup = S.sbuf("mup", [128, 128], F32)
        k.epsT = S.sbuf("epsT", [128, 1], F32)
        k.condT = S.sbuf("condT", [128, NK], F32)
        k.modT = S.sbuf("modT", [128, DEPTH, 96], F32)
        k.A1 = S.sbuf("A1", [128, DEPTH, NK], F32)
        k.A2 = S.sbuf("A2", [128, DEPTH, NK], F32)
        k.nfin = S.sbuf("nfin", [128, NK], F32)
        k.beta = S.sbuf("beta", [128, NCK, 12], F32)
        k.sp = S.sbuf("sp", [128, NCK, 36], F32)
        k.gdec = S.sbuf("gdec", [128, NCK, 36], F32)
        k.cw = S.sbuf("cw", [128, 28, 5], F32)
        k.cb = S.sbuf("cb", [128, 28], F32)
        k.hT = S.sbuf("hT", [128, NK, L], BF16)
        S.dma("sp", I("dma_start", out=k.identF[:], in_=k.c_identF[:]), [k.c_identF], [k.identF])
        S.dma("sp", I("dma_start", out=k.mlow[:], in_=k.c_mlow[:]), [k.c_mlow], [k.mlow])
        S.dma("sp", I("dma_start", out=k.mup[:], in_=k.c_mup[:]), [k.c_mup], [k.mup])
        S.dma("sp", I("dma_start", out=k.nfin[:], in_=k.nfinT[:]), [k.nfinT], [k.nfin])
        S.op("dve", I("tensor_copy", out=k.identB[:], in_=k.identF[:]), [k.identF], [k.identB])
        S.op("dve", I("memset", k.onesF[:], 1.0), [], [k.onesF])
        S.op("dve", I("memset", k.zerosF[:], 0.0), [], [k.zerosF])
        S.op("dve", I("memset", k.epsT[:], EPS), [], [k.epsT])

        phase0(k)
        for l in range(nlayers):
            phase_norm(k, l, 0)
            phase_mixers(k, l)
            phase_outproj(k, l)
            phase_norm(k, l, 1)
            phase_ffn(k, l)
        phase_final(k)
        S.finish()
    _CACHE["used"] = list(k.used)
    return nc


def mm(S, out, lhsT, rhs, start, stop, reads, writes):
    S.op("pe", I("matmul", out, lhsT, rhs, start=start, stop=stop), reads, writes)


def tr(S, out, in_, ident, reads, writes):
    S.op("pe", I("transpose", out, in_, ident), reads, writes)


def phase0(k):
    S = k.S
    with contextlib.ExitStack() as st:
        ada = Rot([S.sbuf(f"p0a{i}", [128, 6 * D], F32, st) for i in range(2)])
        xin = Rot([ada.b[0]])
        xo = Rot([ada.b[1]])
        nm = S.sbuf("p0nm", [128, DEPTH, NK], F32, st)
        nf = S.sbuf("p0nf", [128, DEPTH, NK], F32, st)
        adab = S.sbuf("p0ab", [128, DEPTH, 96], F32, st)
        for tt in range(16):
            xi = xin.next()
            o = xo.next()
            S.dma("sp", I("dma_start", out=xi[:, 0:D], in_=k.x[tt * 128:(tt + 1) * 128, :]), [k.x], [xi])
            for g in range(4):
                ps = k.psrot.next()
                for q in range(4):
                    kk = g * 4 + q
                    tr(S, ps[:, q * 128:(q + 1) * 128], xi[:, kk * 128:(kk + 1) * 128], k.identF[:], [xi, k.identF], [ps])
                src = ps[:].rearrange("p (q t) -> p q t", q=4)
                if g % 2 == 0:
                    S.op("dve", I("tensor_copy", out=o[:, 0:D].rearrange("p (k t) -> p k t", t=128)[:, g * 4:(g + 1) * 4, :], in_=src), [ps], [o])
                else:
                    S.op("act", I("activation", out=o[:, 0:D].rearrange("p (k t) -> p k t", t=128)[:, g * 4:(g + 1) * 4, :], in_=src, func=AF.Copy), [ps], [o])
            S.dma("sp", I("dma_start",
                out=k.xT[:, :, tt * 128:(tt + 1) * 128].rearrange("k p t -> p k t"), in_=o[:, 0:D].rearrange("p (k t) -> p k t", t=128)), [o], [k.xT])
        S.dma("sp", I("dma_start", out=k.condT[:], in_=k.cT[:]), [k.cT], [k.condT])
        S.op("act", I("activation", out=k.condT[:], in_=k.condT[:], func=AF.Silu), [k.condT], [k.condT])
        S.dma("sp", I("dma_start", out=adab[:], in_=k.ada_bT[:]), [k.ada_bT], [adab])
        S.dma("sp", I("dma_start", out=nm[:], in_=k.nmixT[:]), [k.nmixT], [nm])
        S.dma("sp", I("dma_start", out=nf[:], in_=k.nffnT[:]), [k.nffnT], [nf])
        for l in range(DEPTH):
            ps = k.psrot.next()
            mm(S, ps[:, 0:96], k.zerosF[:, :], k.zerosF[:, 0:96], True, False, [k.zerosF], [ps])
            for kk in range(NK):
                blk = ada.next()
                S.dma("sp", I("dma_start", out=blk[:], in_=k.ada_w[l, kk * 128:(kk + 1) * 128, :]), [k.ada_w], [blk])
                for j in range(96):
                    mm(S, ps[:, j:j + 1], blk[:, j * 128:(j + 1) * 128], k.condT[:, kk:kk + 1], False, kk == NK - 1, [blk, k.condT], [ps])
            S.op("dve", I("tensor_tensor", out=k.modT[:, l, :], in0=ps[:, 0:96], in1=adab[:, l, :], op=ALU.add), [ps, adab], [k.modT])
        S.op("dve", I("scalar_tensor_tensor", out=k.A1[:], in0=k.modT[:, :, 16:32], scalar=1.0, in1=nm[:], op0=ALU.add, op1=ALU.mult), [k.modT, nm], [k.A1])
        S.op("dve", I("scalar_tensor_tensor", out=k.A2[:], in0=k.modT[:, :, 64:80], scalar=1.0, in1=nf[:], op0=ALU.add, op1=ALU.mult), [k.modT, nf], [k.A2])
        S.barrier()


def phase_norm(k, l, which):
    S = k.S
    Asc = k.A1 if which == 0 else k.A2
    shoff = 0 if which == 0 else 48
    with contextlib.ExitStack() as st:
        xb = Rot([S.sbuf(f"nx{i}", [128, NK, 512], F32, st) for i in range(2)])
        sq = Rot([S.sbuf(f"nsq{i}", [128, 512], F32, st) for i in range(2)])
        tmp = Rot([S.sbuf(f"ntm{i}", [128, 512], F32, st) for i in range(2)])
        rstd = Rot([S.sbuf(f"nrs{i}", [128, 512], F32, st) for i in range(2)])
        for t in range(4):
            x = xb.next()
            S.dma("sp", I("dma_start", out=x[:], in_=k.xT[:, :, t * 512:(t + 1) * 512].rearrange("k p t -> p k t")), [k.xT], [x])
            ps = k.psrot.next()
            for kk in range(NK):
                s = sq.next()
                S.op("act", I("activation", out=s[:], in_=x[:, kk, :], func=AF.Square), [x], [s])
                mm(S, ps[:], k.onesF[:], s[:], kk == 0, kk == NK - 1, [k.onesF, s], [ps])
            r = rstd.next()
            S.op("act", I("activation", out=r[:], in_=ps[:], func=AF.Sqrt, bias=k.epsT[:], scale=1.0 / D), [ps, k.epsT], [r])
            S.op("dve", I("reciprocal", out=r[:], in_=r[:]), [r], [r])
            for kk in range(NK):
                tm = tmp.next()
                S.op("dve", I("tensor_tensor", out=tm[:], in0=x[:, kk, :], in1=r[:], op=ALU.mult), [x, r], [tm])
                S.op("act", I("activation",
                    out=k.hT[:, kk, t * 512:(t + 1) * 512], in_=tm[:], func=AF.Identity,
                    bias=k.modT[:, l, shoff + kk:shoff + kk + 1], scale=Asc[:, l, kk:kk + 1]), [tm, k.modT, Asc], [k.hT])
        S.barrier()
    if "hT" in k.dbg and which == 0 and l == 0 or "h2dbg" in k.dbg and which == 1 and l == 0:
        S.dma("sp", I("dma_start", out=k.h2T[:].rearrange("k p t -> p k t"), in_=k.hT[:]), [k.hT], [k.h2T])
        S.barrier()


def phase_outproj(k, l):
    S = k.S
    with contextlib.ExitStack() as st:
        wr = Rot([S.sbuf(f"opw{i}", [128, NK, 128], BF16, st) for i in range(2)])
        xr = Rot([S.sbuf(f"opx{i}", [128, 512], F32, st) for i in range(3)])
        for m in range(NK):
            S.dma("sp", I("dma_start", out=k.hT[:, m, :], in_=k.mixT[m * 128:(m + 1) * 128, :]), [k.mixT], [k.hT])
        for j in range(NK):
            w = wr.next()
            S.dma("pool", I("dma_start",
                out=w[:], in_=k.w_out[l, :, j * 128:(j + 1) * 128].rearrange("(m p) c -> p m c", p=128)), [k.w_out], [w])
            for t in range(4):
                ps = k.psrot.next()
                for m in range(NK):
                    mm(S, ps[:], w[:, m, :], k.hT[:, m, t * 512:(t + 1) * 512], m == 0, m == NK - 1, [w, k.hT], [ps])
                xt = xr.next()
                S.dma("sp", I("dma_start", out=xt[:], in_=k.xT[j, :, t * 512:(t + 1) * 512]), [k.xT], [xt])
                S.op("dve", I("scalar_tensor_tensor",
                    out=xt[:], in0=ps[:], scalar=k.modT[:, l, 32 + j:33 + j], in1=xt[:], op0=ALU.mult, op1=ALU.add), [ps, xt, k.modT], [xt])
                S.dma("sp", I("dma_start", out=k.xT[j, :, t * 512:(t + 1) * 512], in_=xt[:]), [xt], [k.xT])
        S.barrier()


def phase_ffn(k, l):
    S = k.S
    moe = (l % 2 == 1)
    li = l // 2
    TH = 1024
    NQ = 11
    FQ = NF // NQ
    with contextlib.ExitStack() as st:
        acc = S.sbuf("facc", [128, NK, TH], F32, st)
        G = S.sbuf("fG", [128, FQ, TH], BF16, st)
        w1r = Rot([S.sbuf(f"fw1{i}", [128, NK, 128], BF16, st) for i in range(2)])
        w3r = Rot([S.sbuf(f"fw3{i}", [128, NK, 128], BF16, st) for i in range(2)])
        w2r = Rot([S.sbuf(f"fw2{i}", [128, FQ, 128], BF16, st) for i in range(2)])
        stg = Rot([S.sbuf(f"fst{i}", [128, NK, 128], F32, st) for i in range(2)])
        stg2 = Rot([S.sbuf(f"fs2{i}", [128, FQ, 128], F32, st) for i in range(2)])
        sar = Rot([S.sbuf(f"fsa{i}", [128, 512], F32, st) for i in range(2)])
        tmr = Rot([S.sbuf(f"ftm{i}", [128, 512], F32, st) for i in range(2)])
        xr = tmr
        if moe:
            rt = S.sbuf("frt", [128, NK, NE], BF16, st)
            lg = S.sbuf("flg", [128, 8, NE], F32, st)
            gate = S.sbuf("fgate", [128, 8, NE], F32, st)
            m8 = S.sbuf("fm8", [128, 8], F32, st)
            sm = S.sbuf("fsm", [128, 4], F32, st)
            selm = S.sbuf("fselm", [128, NE], F32, st)
            ex = S.sbuf("fex", [128, NE], F32, st)
            gT = S.sbuf("fgT", [8, TH], F32, st)
            selT = S.sbuf("fselT", [8, NE, 128], F32, st)
            gbc = Rot([S.sbuf(f"fgbc{i}", [128, TH], F32, st) for i in range(1)])
            S.dma("pool", I("dma_start", out=rt[:], in_=k.moe_router[li].rearrange("(m p) c -> p m c", p=128)), [k.moe_router], [rt])
            S.dma("sp", I("dma_start", out=selT[:], in_=k.c_sel[:]), [k.c_sel], [selT])
        for half in range(2):
            t0 = half * TH
            if moe:
                ps = k.psrot.next()
                for tt in range(8):
                    for kk in range(NK):
                        mm(S, ps[:, tt * NE:(tt + 1) * NE], k.hT[:, kk, t0 + tt * 128:t0 + (tt + 1) * 128], rt[:, kk, :], kk == 0, kk == NK - 1, [k.hT, rt], [ps])
                S.op("dve", I("tensor_copy", out=lg[:], in_=ps[:, 0:8 * NE].rearrange("p (t e) -> p t e", e=NE)), [ps], [lg])
                psT = k.psrot.next()
                for tt in range(8):
                    S.op("dve", I("max", out=m8[:], in_=lg[:, tt, :]), [lg], [m8])
                    S.op("dve", I("tensor_scalar", out=selm[:], in0=lg[:, tt, :], scalar1=m8[:, 1:2], scalar2=None, op0=ALU.is_ge), [lg, m8], [selm])
                    S.op("dve", I("tensor_scalar", out=sm[:, 0:1], in0=m8[:, 0:1], scalar1=-1.0, scalar2=None, op0=ALU.mult), [m8], [sm])
                    S.op("act", I("activation", out=ex[:], in_=lg[:, tt, :], func=AF.Exp, bias=sm[:, 0:1], scale=1.0), [lg, sm], [ex])
                    S.op("dve", I("tensor_tensor", out=ex[:], in0=ex[:], in1=selm[:], op=ALU.mult), [ex, selm], [ex])
                    S.op("dve", I("reduce_sum", out=sm[:, 1:2], in_=ex[:], axis=AX.X), [ex], [sm])
                    S.op("dve", I("reciprocal", out=sm[:, 2:3], in_=sm[:, 1:2]), [sm], [sm])
                    S.op("dve", I("tensor_scalar", out=gate[:, tt, :], in0=ex[:], scalar1=sm[:, 2:3], scalar2=None, op0=ALU.mult), [ex, sm], [gate])
                    tr(S, psT[0:8, tt * 128:(tt + 1) * 128] if tt < 4 else psT[0:8, (tt - 4) * 128:(tt - 3) * 128], gate[:, tt, :], k.identF[:], [gate, k.identF], [psT])
                    if tt == 3 or tt == 7:
                        hh = 0 if tt == 3 else 1
                        S.op("dve", I("tensor_copy", out=gT[:, hh * 512:(hh + 1) * 512], in_=psT[0:8, :]), [psT], [gT])
                        if tt == 3:
                            psT = k.psrot.next()
            nexp = NE if moe else 1
            first = True
            for ex_i in range(nexp):
                if moe:
                    W1, W3, W2 = k.moe_w1[li, ex_i], k.moe_w3[li, ex_i], k.moe_w2[li, ex_i]
                    gb = gbc.next()
                    for t in range(2):
                        ps = k.psrot.next()
                        mm(S, ps[:], selT[:, ex_i, :], gT[:, t * 512:(t + 1) * 512], True, True, [selT, gT], [ps])
                        S.op("act", I("activation", out=gb[:, t * 512:(t + 1) * 512], in_=ps[:], func=AF.Copy), [ps], [gb])
                else:
                    W1, W3, W2 = k.ffn_w1[li], k.ffn_w3[li], k.ffn_w2[li]
                for q in range(NQ):
                    for fi in range(FQ):
                        f = q * FQ + fi
                        w1 = w1r.next()
                        w3 = w3r.next()
                        s1 = stg.next()
                        S.dma("sp", I("dma_start", out=s1[:], in_=W1[:, f * 128:(f + 1) * 128].rearrange("(m p) c -> p m c", p=128)), [k.moe_w1 if moe else k.ffn_w1], [s1])
                        S.op("act", I("activation", out=w1[:], in_=s1[:], func=AF.Copy), [s1], [w1])
                        s3 = stg.next()
                        S.dma("sp", I("dma_start", out=s3[:], in_=W3[:, f * 128:(f + 1) * 128].rearrange("(m p) c -> p m c", p=128)), [k.moe_w3 if moe else k.ffn_w3], [s3])
                        S.op("act", I("activation", out=w3[:], in_=s3[:], func=AF.Copy), [s3], [w3])
                        for t in range(2):
                            pa = k.psrot.next()
                            pb = k.psrot.next()
                            for kk in range(NK):
                                mm(S, pa[:], w1[:, kk, :], k.hT[:, kk, t0 + t * 512:t0 + (t + 1) * 512], kk == 0, kk == NK - 1, [w1, k.hT], [pa])
                            for kk in range(NK):
                                mm(S, pb[:], w3[:, kk, :], k.hT[:, kk, t0 + t * 512:t0 + (t + 1) * 512], kk == 0, kk == NK - 1, [w3, k.hT], [pb])
                            sa = sar.next()
                            S.op("act", I("activation", out=sa[:], in_=pa[:], func=AF.Silu), [pa], [sa])
                            S.op("dve", I("tensor_tensor", out=G[:, fi, t * 512:(t + 1) * 512], in0=pb[:], in1=sa[:], op=ALU.mult), [pb, sa], [G])
                    for j in range(NK):
                        w2 = w2r.next()
                        s2 = stg2.next()
                        S.dma("sp", I("dma_start",
                            out=s2[:], in_=W2[q * FQ * 128:(q + 1) * FQ * 128, j * 128:(j + 1) * 128].rearrange("(i p) c -> p i c", p=128)), [k.moe_w2 if moe else k.ffn_w2], [s2])
                        S.op("act", I("activation", out=w2[:], in_=s2[:], func=AF.Copy), [s2], [w2])
                        for t in range(2):
                            ps = k.psrot.next()
                            for fi in range(FQ):
                                mm(S, ps[:], w2[:, fi, :], G[:, fi, t * 512:(t + 1) * 512], fi == 0, fi == FQ - 1, [w2, G], [ps])
                            dst = acc[:, j, t * 512:(t + 1) * 512]
                            if moe:
                                if first:
                                    S.op("dve", I("tensor_tensor", out=dst, in0=ps[:], in1=gb[:, t * 512:(t + 1) * 512], op=ALU.mult), [ps, gb], [acc])
                                else:
                                    tm = tmr.next()
                                    S.op("dve", I("tensor_tensor", out=tm[:], in0=ps[:], in1=gb[:, t * 512:(t + 1) * 512], op=ALU.mult), [ps, gb], [tm])
                                    S.op("pool", I("tensor_tensor", out=dst, in0=dst, in1=tm[:], op=ALU.add), [tm, acc], [acc])
                            else:
                                if first:
                                    S.op("act", I("activation", out=dst, in_=ps[:], func=AF.Copy), [ps], [acc])
                                else:
                                    S.op("dve", I("tensor_tensor", out=dst, in0=ps[:], in1=dst, op=ALU.add), [ps, acc], [acc])
                    first = False
            if "modT" in k.dbg and half == 0 and l == 0:
                S.dma("sp", I("dma_start", out=k.dacc[:], in_=acc[:].rearrange("p a b -> p (a b)")), [acc], [k.dacc])
                S.dma("sp", I("dma_start", out=k.dmod[:], in_=k.modT[:].rearrange("p a b -> p (a b)")), [k.modT], [k.dmod])
                S.dma("sp", I("dma_start", out=k.dh2[:], in_=k.hT[:].rearrange("p a b -> p (a b)")), [k.hT], [k.dh2])
            for j in range(NK):
                for t in range(2):
                    xt = xr.next()
                    S.dma("sp", I("dma_start", out=xt[:], in_=k.xT[j, :, t0 + t * 512:t0 + (t + 1) * 512]), [k.xT], [xt])
                    S.op("dve", I("scalar_tensor_tensor",
                        out=xt[:], in0=acc[:, j, t * 512:(t + 1) * 512], scalar=k.modT[:, l, 80 + j:81 + j], in1=xt[:], op0=ALU.mult, op1=ALU.add), [acc, xt, k.modT], [xt])
                    S.dma("sp", I("dma_start", out=k.xT[j, :, t0 + t * 512:t0 + (t + 1) * 512], in_=xt[:]), [xt], [k.xT])
        S.barrier()


def phase_final(k):
    S = k.S
    with contextlib.ExitStack() as st:
        xb = Rot([S.sbuf(f"zx{i}", [128, NK, 512], F32, st) for i in range(2)])
        sq = Rot([S.sbuf(f"zsq{i}", [128, 512], F32, st) for i in range(2)])
        rstd = S.sbuf("zrs", [128, 512], F32, st)
        ob = Rot([S.sbuf(f"zo{i}", [128, D], F32, st) for i in range(2)])
        for t in range(4):
            x = xb.next()
            S.dma("sp", I("dma_start", out=x[:], in_=k.xT[:, :, t * 512:(t + 1) * 512].rearrange("k p t -> p k t")), [k.xT], [x])
            ps = k.psrot.next()
            for kk in range(NK):
                s = sq.next()
                S.op("act", I("activation", out=s[:], in_=x[:, kk, :], func=AF.Square), [x], [s])
                mm(S, ps[:], k.onesF[:], s[:], kk == 0, kk == NK - 1, [k.onesF, s], [ps])
            S.op("act", I("activation", out=rstd[:], in_=ps[:], func=AF.Sqrt, bias=k.epsT[:], scale=1.0 / D), [ps, k.epsT], [rstd])
            S.op("dve", I("reciprocal", out=rstd[:], in_=rstd[:]), [rstd], [rstd])
            for kk in range(NK):
                S.op("dve", I("scalar_tensor_tensor",
                    out=x[:, kk, :], in0=x[:, kk, :], scalar=k.nfin[:, kk:kk + 1], in1=rstd[:], op0=ALU.mult, op1=ALU.mult), [x, k.nfin, rstd], [x])
            for tb in range(4):
                o = ob.next()
                for g in range(4):
                    ps2 = k.psrot.next()
                    for q in range(4):
                        kk = g * 4 + q
                        tr(S, ps2[:, q * 128:(q + 1) * 128], x[:, kk, tb * 128:(tb + 1) * 128], k.identF[:], [x, k.identF], [ps2])
                    if g % 2 == 0:
                        S.op("dve", I("tensor_copy", out=o[:, g * 512:(g + 1) * 512], in_=ps2[:]), [ps2], [o])
                    else:
                        S.op("act", I("activation", out=o[:, g * 512:(g + 1) * 512], in_=ps2[:], func=AF.Copy), [ps2], [o])
                r0 = t * 512 + tb * 128
                S.dma("sp", I("dma_start", out=k.out[r0:r0 + 128, :], in_=o[:]), [o], [k.out])
        S.barrier()


def evac(S, i, out, in_, reads, writes):
    if i % 2 == 0:
        S.op("dve", I("tensor_copy", out=out, in_=in_), reads, writes)
    else:
        S.op("act", I("activation", out=out, in_=in_, func=AF.Copy), reads, writes)


def phase_mixers(k, l):
    S = k.S
    with contextlib.ExitStack() as st:
        if not all(c in k.flags for c in "gsn"):
            z = S.sbuf("mxz", [128, L], BF16, st)
            S.op("dve", I("memset", z[:], 0.0), [], [z])
            for m in range(NK):
                S.dma("sp", I("dma_start", out=k.mixT[m * 128:(m + 1) * 128, :], in_=z[:]), [z], [k.mixT])
            S.barrier()
    if "g" in k.flags or "s" in k.flags:
        phase_gates(k, l)
    if "g" in k.flags:
        mixer_gdn(k, l)
    if "s" in k.flags:
        mixer_ssd(k, l)
    if "n" in k.flags:
        mixer_na(k, l)


def mixer_na(k, l):
    S = k.S
    rot6 = Rot(k.PS[0:6])
    accr = Rot(k.PS[6:8])
    with contextlib.ExitStack() as st:
        wv = S.sbuf("nawv", [128, NK, 512], BF16, st)
        vT = S.sbuf("navt", [64, 32, 512], BF16, st)
        wq = Rot([S.sbuf(f"nawq{i}", [128, NK, 64], BF16, st) for i in range(2)])
        qT = S.sbuf("naq", [64, L], BF16, st)
        kT = S.sbuf("nak", [64, L], BF16, st)
        rb = S.sbuf("narb", [64, 15 * 64], F32, st)
        msk = S.sbuf("namsk", [64, 64], F32, st)
        sbr = Rot([S.sbuf(f"nas{i}", [64, 512], F32, st) for i in range(3)])
        pbr = Rot([S.sbuf(f"nap{i}", [64, 512], BF16, st) for i in range(3)])
        ptr_ = Rot([S.sbuf(f"napt{i}", [64, 8, 64], BF16, st) for i in range(3)])
        smr = Rot([S.sbuf(f"nasm{i}", [64, 4], F32, st) for i in range(4)])
        yc = S.sbuf("nayc", [64, L], BF16, st)
        S.dma("pool", I("dma_start", out=wv[:], in_=k.w_in[l, :, C_NV:C_NV + 512].rearrange("(m p) c -> p m c", p=128)), [k.w_in], [wv])
        S.dma("sp", I("dma_start", out=msk[:], in_=k.c_namask[:]), [k.c_namask], [msk])
        for i in range(32):
            ps = rot6.next()
            for kk in range(NK):
                mm(S, ps[0:64, :], k.hT[:, kk, i * 64:(i + 1) * 64], wv[:, kk, :], kk == 0, kk == NK - 1, [k.hT, wv], [ps])
            evac(S, i, vT[:, i, :], ps[0:64, :], [ps], [vT])
        for h in range(8):
            for dst, c0 in ((qT, C_NQ + h * 64), (kT, C_NK + h * 64)):
                w = wq.next()
                S.dma("pool", I("dma_start", out=w[:], in_=k.w_in[l, :, c0:c0 + 64].rearrange("(m p) c -> p m c", p=128)), [k.w_in], [w])
                for t in range(4):
                    ps = rot6.next()
                    for kk in range(NK):
                        mm(S, ps[0:64, :], w[:, kk, :], k.hT[:, kk, t * 512:(t + 1) * 512], kk == 0, kk == NK - 1, [w, k.hT], [ps])
                    evac(S, t, dst[:, t * 512:(t + 1) * 512], ps[0:64, :], [ps], [dst])
            S.dma("sp", I("dma_start", out=rb[:], in_=k.na_rb[l, h]), [k.na_rb], [rb])
            S.op("dve", I("tensor_tensor", out=rb[:].rearrange("p (d c) -> p d c", c=64), in0=rb[:].rearrange("p (d c) -> p d c", c=64),
                                                  in1=msk[:].unsqueeze(1).broadcast_to([64, 15, 64]), op=ALU.add), [rb, msk], [rb])
            po = None
            for r in range(32):
                rs = min(max(r - 4, 0), 24)
                dr0 = rs - r + 7
                ps = rot6.next()
                mm(S, ps[0:64, :], qT[:, r * 64:(r + 1) * 64], kT[:, rs * 64:rs * 64 + 512], True, True, [qT, kT], [ps])
                s = sbr.next()
                sm = smr.next()
                S.op("dve", I("scalar_tensor_tensor",
                    out=s[:], in0=ps[0:64, :], scalar=0.125, in1=rb[:, dr0 * 64:dr0 * 64 + 512], op0=ALU.mult, op1=ALU.add), [ps, rb], [s])
                S.op("dve", I("reduce_max", out=sm[:, 0:1], in_=s[:], axis=AX.X), [s], [sm])
                S.op("dve", I("tensor_scalar", out=sm[:, 1:2], in0=sm[:, 0:1], scalar1=-1.0, scalar2=None, op0=ALU.mult), [sm], [sm])
                S.op("act", I("activation", out=s[:], in_=s[:], func=AF.Exp, bias=sm[:, 1:2], scale=1.0, accum_out=sm[:, 2:3]), [s, sm], [s, sm])
                S.op("dve", I("reciprocal", out=sm[:, 3:4], in_=sm[:, 2:3]), [sm], [sm])
                p_ = pbr.next()
                S.op("dve", I("tensor_scalar", out=p_[:], in0=s[:], scalar1=sm[:, 3:4], scalar2=None, op0=ALU.mult), [s, sm], [p_])
                pst = rot6.next()
                pstb = pst[:].bitcast(BF16)
                for i in range(8):
                    tr(S, pstb[0:64, i * 64:(i + 1) * 64], p_[:, i * 64:(i + 1) * 64], k.identB[0:64, 0:64], [p_, k.identB], [pst])
                pt = ptr_.next()
                S.op("act", I("activation", out=pt[:].rearrange("p a b -> p (a b)"), in_=pstb[0:64, 0:512], func=AF.Copy), [pst], [pt])
                if r % 8 == 0:
                    po = accr.next()
                for i in range(8):
                    mm(S, po[0:64, (r % 8) * 64:(r % 8 + 1) * 64], vT[:, rs + i, h * 64:(h + 1) * 64], pt[:, i, :], i == 0, i == 7, [vT, pt], [po])
                if r % 8 == 7:
                    evac(S, r // 8, yc[:, (r - 7) * 64:(r + 1) * 64], po[0:64, :], [po], [yc])
            S.dma("sp", I("dma_start", out=k.mixT[1536 + h * 64:1536 + (h + 1) * 64, :], in_=yc[:]), [yc], [k.mixT])
        S.barrier()


def phase_gates(k, l):
    S = k.S
    with contextlib.ExitStack() as st:
        wg = S.sbuf("gwg", [128, NK, 48], BF16, st)
        GT = S.sbuf("gGT", [128, NCK, 48], F32, st)
        gbias = S.sbuf("ggb", [128, 48], F32, st)
        galog = S.sbuf("gga", [128, 48], F32, st)
        S.dma("pool", I("dma_start", out=wg[:, :, 0:24], in_=k.w_in[l, :, C_GB:C_GB + 24].rearrange("(m p) c -> p m c", p=128)), [k.w_in], [wg])
        S.dma("pool", I("dma_start", out=wg[:, :, 24:48], in_=k.w_in[l, :, C_SDT:C_SDT + 24].rearrange("(m p) c -> p m c", p=128)), [k.w_in], [wg])
        S.dma("sp", I("dma_start", out=gbias[:], in_=k.gate_bias[:, l, :]), [k.gate_bias], [gbias])
        S.dma("sp", I("dma_start", out=galog[:], in_=k.gate_alog[:, l, :]), [k.gate_alog], [galog])
        for half in range(2):
            ps = k.psrot.next()
            for i in range(8):
                n = half * 8 + i
                for kk in range(NK):
                    mm(S, ps[:, i * 48:(i + 1) * 48], k.hT[:, kk, n * 128:(n + 1) * 128], wg[:, kk, :], kk == 0, kk == NK - 1, [k.hT, wg], [ps])
            S.op("dve", I("tensor_tensor",
                out=GT[:, half * 8:(half + 1) * 8, :], in0=ps[:, 0:384].rearrange("p (n c) -> p n c", c=48),
                in1=gbias[:].unsqueeze(1).broadcast_to([128, 8, 48]), op=ALU.add), [ps, gbias], [GT])
        S.op("act", I("activation", out=k.beta[:], in_=GT[:, :, 0:12], func=AF.Sigmoid), [GT], [k.beta])
        S.op("act", I("activation", out=k.sp[:], in_=GT[:, :, 12:48], func=AF.Exp), [GT], [k.sp])
        S.op("act", I("activation", out=k.sp[:], in_=k.sp[:], func=AF.Ln, bias=k.onesF[:, 0:1], scale=1.0), [k.sp, k.onesF], [k.sp])
        S.op("act", I("activation", out=galog[:], in_=galog[:], func=AF.Exp), [galog], [galog])
        S.op("dve", I("scalar_tensor_tensor", out=k.gdec[:], in0=k.sp[:], scalar=-1.0, in1=galog[:, 12:48].unsqueeze(1).broadcast_to([128, NCK, 36]),
                                                     op0=ALU.mult, op1=ALU.mult), [k.sp, galog], [k.gdec])
        S.barrier()


def conv_chunk(k, T, l, ci, out_ap, out_buf, rot4):
    S = k.S
    w = T["w"].next()
    S.dma("pool", I("dma_start", out=w[:], in_=k.w_in[l, :, ci * 128:(ci + 1) * 128].rearrange("(m p) c -> p m c", p=128)), [k.w_in], [w])
    xp = T["xp"]
    acc = T["acc"]
    for t in range(4):
        ps = rot4.next()
        for kk in range(NK):
            mm(S, ps[:], w[:, kk, :], k.hT[:, kk, t * 512:(t + 1) * 512], kk == 0, kk == NK - 1, [w, k.hT], [ps])
        evac(S, t, xp[:, 2 + t * 512:2 + (t + 1) * 512], ps[:], [ps], [xp])
    S.op("dve", I("tensor_scalar", out=acc[:], in0=xp[:, 0:L], scalar1=k.cw[:, ci, 0:1], scalar2=None, op0=ALU.mult), [xp, k.cw], [acc])
    for j in range(1, 5):
        S.op("dve", I("scalar_tensor_tensor", out=acc[:], in0=xp[:, j:j + L], scalar=k.cw[:, ci, j:j + 1], in1=acc[:], op0=ALU.mult, op1=ALU.add), [xp, k.cw, acc], [acc])
    S.op("act", I("activation", out=out_ap, in_=acc[:], func=AF.Silu, bias=k.cb[:, ci:ci + 1], scale=1.0), [acc, k.cb], [out_buf])


def conv_temps(k, st, l):
    S = k.S
    T = {"w": Rot([S.sbuf(f"cvw{i}", [128, NK, 128], BF16, st) for i in range(2)]),
         "xp": S.sbuf("cvxp", [128, L + 4], F32, st), "acc": S.sbuf("cvacc", [128, L], F32, st)}
    S.op("dve", I("memset", T["xp"][:, 0:2], 0.0), [], [T["xp"]])
    S.op("dve", I("memset", T["xp"][:, L + 2:L + 4], 0.0), [], [T["xp"]])
    return T


def load_conv_params(k, l):
    S = k.S
    S.dma("sp", I("dma_start", out=k.cw[:], in_=k.conv_wT[:, l, :, :]), [k.conv_wT], [k.cw])
    S.dma("sp", I("dma_start", out=k.cb[:], in_=k.conv_bT[:, l, :]), [k.conv_bT], [k.cb])


def decay_prep(k, T, gview, d, rot4):
    S = k.S
    tri = k.mup if d == 0 else k.mlow
    g, gc, egc, egl, ekd, R, ET = T["g"], T["gc"], T["egc"], T["egl"], T["ekd"], T["R"], T["ET"]
    S.op("dve", I("tensor_copy", out=g[:], in_=gview), [k.gdec], [g])
    p1 = rot4.next()
    mm(S, p1[:, 0:NCK], tri[:], g[:], True, True, [tri, g], [p1])
    mm(S, p1[:, 64:64 + NCK], k.onesF[:], g[:], True, True, [k.onesF, g], [p1])
    S.op("dve", I("tensor_copy", out=gc[:], in_=p1[:, 0:NCK]), [p1], [gc])
    S.op("act", I("activation", out=egc[:], in_=p1[:, 0:NCK], func=AF.Exp), [p1], [egc])
    S.op("act", I("activation", out=egl[:], in_=p1[:, 64:64 + NCK], func=AF.Exp), [p1], [egl])
    S.op("dve", I("tensor_tensor", out=ekd[:], in0=p1[:, 64:64 + NCK], in1=gc[:], op=ALU.subtract), [p1, gc], [ekd])
    S.op("act", I("activation", out=ekd[:], in_=ekd[:], func=AF.Exp), [ekd], [ekd])
    S.op("pool", I("tensor_tensor", out=R[:].rearrange("p (n s) -> p n s", s=CH), in0=tri[:].unsqueeze(1).broadcast_to([128, NCK, CH]),
                                           in1=g[:].unsqueeze(2).broadcast_to([128, NCK, CH]), op=ALU.mult), [tri, g], [R])
    valid = k.mup if d == 0 else k.mlow
    for t in range(4):
        pb = rot4.next()
        mm(S, pb[:], k.onesF[:], R[:, t * 512:(t + 1) * 512], True, True, [k.onesF, R], [pb])
        S.op("dve", I("tensor_tensor",
            out=ET[:, t * 512:(t + 1) * 512].rearrange("p (n s) -> p n s", s=CH), in0=pb[:].rearrange("p (n s) -> p n s", s=CH),
            in1=gc[:, t * 4:(t + 1) * 4].unsqueeze(2).broadcast_to([128, 4, CH]), op=ALU.subtract), [pb, gc], [ET])
    S.op("dve", I("tensor_scalar", out=ET[:], in0=ET[:], scalar1=0.0, scalar2=None, op0=ALU.min), [ET], [ET])
    S.op("act", I("activation", out=ET[:], in_=ET[:], func=AF.Exp), [ET], [ET])
    S.op("pool", I("tensor_tensor", out=ET[:].rearrange("p (n s) -> p n s", s=CH), in0=ET[:].rearrange("p (n s) -> p n s", s=CH),
                                           in1=valid[:].unsqueeze(1).broadcast_to([128, NCK, CH]), op=ALU.mult), [ET, valid], [ET])
    for b in (ET, egc, egl, ekd):
        S.op("dve", I("tensor_scalar", out=b[:], in0=b[:], scalar1=1.0, scalar2=1.0, op0=ALU.add, op1=ALU.subtract), [b], [b])


def decay_temps(k, st):
    S = k.S
    T = {n: S.sbuf("dc" + n, [128, NCK], F32, st) for n in ("g", "gc", "egc", "egl", "ekd")}
    T["R"] = S.sbuf("dcR", [128, L], F32, st)
    T["ET"] = S.sbuf("dcET", [128, L], F32, st)
    return T


def mixer_ssd(k, l):
    S = k.S
    rot4 = Rot(k.PS[0:5])
    pA_r, pB_r, pC_r = k.PS[5], k.PS[6], k.PS[7]
    load_conv_params(k, l)
    with contextlib.ExitStack() as st0:
        ssq = S.sbuf("sdssq", [128, 2, NCK], F32, st0)
        dsk = S.sbuf("sddsk", [128, 12], F32, st0)
        nrm = S.sbuf("sdnrm", [128, 768], F32, st0)
        S.dma("sp", I("dma_start", out=dsk[:], in_=k.ssm_Dbc[:, l, :]), [k.ssm_Dbc], [dsk])
        S.dma("sp", I("dma_start", out=nrm[:], in_=k.ssm_normbc[:, l, :]), [k.ssm_normbc], [nrm])
        for g in range(2):
            with contextlib.ExitStack() as st:
                CT = S.sbuf("sdCT", [128, L], BF16, st)
                BT = S.sbuf("sdBT", [128, L], BF16, st)
                BTM = S.sbuf("sdBTM", [128, NCK, 128], BF16, st)
                xTM = S.sbuf("sdxTM", [128, NCK, 384], BF16, st)
                CBT = S.sbuf("sdCBT", [128, L], F32, st)
                yacc = S.sbuf("sdyacc", [128, NCK, 384], F32, st)
                with contextlib.ExitStack() as sta:
                    T = conv_temps(k, sta, l)
                    xfm = S.sbuf("sdxfm", [128, L], BF16, sta)
                    conv_chunk(k, T, l, 24 + g, BT[:], BT, rot4)
                    conv_chunk(k, T, l, 26 + g, CT[:], CT, rot4)
                    for n in range(NCK):
                        if n % 4 == 0:
                            pt = rot4.next()
                            ptb = pt[:].bitcast(BF16)
                        tr(S, ptb[:, (n % 4) * 128:(n % 4 + 1) * 128], BT[:, n * 128:(n + 1) * 128], k.identB[:], [BT, k.identB], [pt])
                        if n % 4 == 3:
                            evac(S, n // 4, BTM[:, n - 3:n + 1, :].rearrange("p a b -> p (a b)"), ptb[:, 0:512], [pt], [BTM])
                    for i in range(3):
                        conv_chunk(k, T, l, 18 + 3 * g + i, xfm[:], xfm, rot4)
                        for n in range(NCK):
                            if n % 4 == 0:
                                pt = rot4.next()
                                ptb = pt[:].bitcast(BF16)
                            tr(S, ptb[:, (n % 4) * 128:(n % 4 + 1) * 128], xfm[:, n * 128:(n + 1) * 128], k.identB[:], [xfm, k.identB], [pt])
                            if n % 4 == 3:
                                evac(S, n // 4, xTM[:, n - 3:n + 1, i * 128:(i + 1) * 128], ptb[:, 0:512].rearrange("p (a b) -> p a b", b=128), [pt], [xTM])
                    for n in range(NCK):
                        if n % 4 == 0:
                            pc = rot4.next()
                        mm(S, pc[:, (n % 4) * 128:(n % 4 + 1) * 128], BT[:, n * 128:(n + 1) * 128], CT[:, n * 128:(n + 1) * 128], True, True, [BT, CT], [pc])
                        if n % 4 == 3:
                            evac(S, n // 4, CBT[:, (n - 3) * 128:(n + 1) * 128], pc[:], [pc], [CBT])
                    S.barrier()
                for d in range(2):
                    with contextlib.ExitStack() as stb:
                        T = decay_temps(k, stb)
                        xdt = S.sbuf("sdxdt", [128, NCK, 384], BF16, stb)
                        xdts = S.sbuf("sdxdts", [128, NCK, 384], BF16, stb)
                        M = [S.sbuf(f"sdM{h}", [128, L], BF16, stb) for h in range(6)]
                        eac = S.sbuf("sdeac", [128, NCK, 6], F32, stb)
                        eal = S.sbuf("sdeal", [128, NCK, 6], F32, stb)
                        eks = S.sbuf("sdeks", [128, NCK, 6], F32, stb)
                        S32 = S.sbuf("sdS32", [128, 384], F32, stb)
                        Sbf = S.sbuf("sdSbf", [128, 384], BF16, stb)
                        tmr = Rot([S.sbuf(f"sdtm{i}", [128, 384], F32, stb) for i in range(2)])
                        tm2 = Rot([S.sbuf(f"sdtn{i}", [128, 384], F32, stb) for i in range(2)])
                        for h in range(6):
                            col = 12 + d * 12 + g * 6 + h
                            decay_prep(k, T, k.gdec[:, :, col], d, rot4)
                            S.op("dve", I("tensor_tensor", out=M[h][:], in0=CBT[:], in1=T["ET"][:], op=ALU.mult), [CBT, T["ET"]], [M[h]])
                            S.op("dve", I("tensor_copy", out=eac[:, :, h], in_=T["egc"][:]), [T["egc"]], [eac])
                            S.op("dve", I("tensor_copy", out=eal[:, :, h], in_=T["egl"][:]), [T["egl"]], [eal])
                            S.op("dve", I("tensor_copy", out=eks[:, :, h], in_=T["ekd"][:]), [T["ekd"]], [eks])
                        dtv = k.sp[:, :, 12 + d * 12 + g * 6:12 + d * 12 + g * 6 + 6]
                        S.op("dve", I("tensor_tensor", out=xdt[:].rearrange("p n (h q) -> p n h q", q=64), in0=xTM[:].rearrange("p n (h q) -> p n h q", q=64),
                                                              in1=dtv.unsqueeze(3).broadcast_to([128, NCK, 6, 64]), op=ALU.mult), [xTM, k.sp], [xdt])
                        S.op("dve", I("tensor_tensor", out=xdts[:].rearrange("p n (h q) -> p n h q", q=64), in0=xdt[:].rearrange("p n (h q) -> p n h q", q=64),
                                                              in1=eks[:].unsqueeze(3).broadcast_to([128, NCK, 6, 64]), op=ALU.mult), [xdt, eks], [xdts])
                        S.op("dve", I("memset", S32[:], 0.0), [], [S32])
                        S.op("dve", I("memset", Sbf[:], 0.0), [], [Sbf])
                        order = range(NCK) if d == 0 else range(NCK - 1, -1, -1)
                        for n in order:
                            mm(S, pA_r[:, 0:384], CT[:, n * 128:(n + 1) * 128], Sbf[:], True, True, [CT, Sbf], [pA_r])
                            for h in range(6):
                                mm(S, pB_r[:, h * 64:(h + 1) * 64], M[h][:, n * 128:(n + 1) * 128], xdt[:, n, h * 64:(h + 1) * 64], True, True, [M[h], xdt], [pB_r])
                            mm(S, pC_r[:, 0:384], BTM[:, n, :], xdts[:, n, :], True, True, [BTM, xdts], [pC_r])
                            tm = tmr.next()
                            S.op("dve", I("tensor_tensor", out=tm[:].rearrange("p (h q) -> p h q", q=64), in0=pA_r[:, 0:384].rearrange("p (h q) -> p h q", q=64),
                                                                               in1=eac[:, n, :].unsqueeze(2).broadcast_to([128, 6, 64]), op=ALU.mult), [pA_r, eac], [tm])
                            if d == 0:
                                S.op("dve", I("tensor_tensor", out=yacc[:, n, :], in0=pB_r[:, 0:384], in1=tm[:], op=ALU.add), [pB_r, tm], [yacc])
                            else:
                                t2 = tm2.next()
                                S.op("dve", I("tensor_tensor", out=t2[:], in0=pB_r[:, 0:384], in1=tm[:], op=ALU.add), [pB_r, tm], [t2])
                                S.op("pool", I("tensor_tensor", out=yacc[:, n, :], in0=yacc[:, n, :], in1=t2[:], op=ALU.add), [t2, yacc], [yacc])
                            S.op("pool", I("tensor_tensor", out=S32[:].rearrange("p (h q) -> p h q", q=64), in0=S32[:].rearrange("p (h q) -> p h q", q=64),
                                                                         in1=eal[:, n, :].unsqueeze(2).broadcast_to([128, 6, 64]), op=ALU.mult), [S32, eal], [S32])
                            S.op("dve", I("tensor_tensor", out=S32[:], in0=pC_r[:, 0:384], in1=S32[:], op=ALU.add), [pC_r, S32], [S32])
                            S.op("act", I("activation", out=Sbf[:], in_=S32[:], func=AF.Copy), [S32], [Sbf])
                        S.barrier()
                with contextlib.ExitStack() as stc:
                    wz = S.sbuf("sdwz", [128, NK, 384], BF16, stc)
                    szr = Rot([S.sbuf(f"sdsz{i}", [128, 384], F32, stc) for i in range(2)])
                    t1r = Rot([S.sbuf(f"sdt1{i}", [128, 384], F32, stc) for i in range(2)])
                    S.dma("pool", I("dma_start", out=wz[:], in_=k.w_in[l, :, C_SZ + g * 384:C_SZ + (g + 1) * 384].rearrange("(m p) c -> p m c", p=128)), [k.w_in], [wz])
                    for n in range(NCK):
                        pz = rot4.next()
                        for kk in range(NK):
                            mm(S, pz[:, 0:384], k.hT[:, kk, n * 128:(n + 1) * 128], wz[:, kk, :], kk == 0, kk == NK - 1, [k.hT, wz], [pz])
                        sz = szr.next()
                        t1 = t1r.next()
                        S.op("act", I("activation", out=sz[:], in_=pz[:, 0:384], func=AF.Silu), [pz], [sz])
                        S.op("dve", I("tensor_tensor", out=t1[:].rearrange("p (h q) -> p h q", q=64), in0=xTM[:, n, :].rearrange("p (h q) -> p h q", q=64),
                                                                           in1=dsk[:, g * 6:(g + 1) * 6].unsqueeze(2).broadcast_to([128, 6, 64]), op=ALU.mult), [xTM, dsk], [t1])
                        S.op("dve", I("tensor_tensor", out=t1[:], in0=t1[:], in1=yacc[:, n, :], op=ALU.add), [t1, yacc], [t1])
                        S.op("pool", I("tensor_tensor", out=yacc[:, n, :], in0=t1[:], in1=sz[:], op=ALU.mult), [t1, sz, yacc], [yacc])
                        S.op("act", I("activation", out=t1[:], in_=yacc[:, n, :], func=AF.Square, accum_out=ssq[:, g, n:n + 1]), [yacc], [t1, ssq])
                    S.dma("sp", I("dma_start", out=k.yss[g], in_=yacc[:].rearrange("p a b -> p (a b)")), [yacc], [k.yss])
                    S.barrier()
        with contextlib.ExitStack() as st:
            rstd = S.sbuf("sdrstd", [128, NCK], F32, st)
            yz = S.sbuf("sdyz", [128, NCK, 384], F32, st)
            ybr = Rot([S.sbuf(f"sdyb{i}", [128, 384], BF16, st) for i in range(2)])
            yT = S.sbuf("sdyT", [128, 3, L], BF16, st)
            S.op("dve", I("tensor_tensor", out=rstd[:], in0=ssq[:, 0, :], in1=ssq[:, 1, :], op=ALU.add), [ssq], [rstd])
            S.op("act", I("activation", out=rstd[:], in_=rstd[:], func=AF.Sqrt, bias=k.epsT[:], scale=1.0 / 768), [rstd, k.epsT], [rstd])
            S.op("dve", I("reciprocal", out=rstd[:], in_=rstd[:]), [rstd], [rstd])
            for g in range(2):
                S.dma("sp", I("dma_start", out=yz[:].rearrange("p a b -> p (a b)"), in_=k.yss[g]), [k.yss], [yz])
                for n in range(NCK):
                    yb = ybr.next()
                    S.op("dve", I("scalar_tensor_tensor", out=yb[:], in0=yz[:, n, :], scalar=rstd[:, n:n + 1], in1=nrm[:, g * 384:(g + 1) * 384],
                                                                               op0=ALU.mult, op1=ALU.mult), [yz, rstd, nrm], [yb])
                    pt = rot4.next()
                    ptb = pt[:].bitcast(BF16)
                    for i in range(3):
                        tr(S, ptb[:, i * 128:(i + 1) * 128], yb[:, i * 128:(i + 1) * 128], k.identB[:], [yb, k.identB], [pt])
                    evac(S, n, yT[:, :, n * 128:(n + 1) * 128], ptb[:, 0:384].rearrange("p (a b) -> p a b", b=128), [pt], [yT])
                for i in range(3):
                    r0 = 768 + g * 384 + i * 128
                    S.dma("sp", I("dma_start", out=k.mixT[r0:r0 + 128, :], in_=yT[:, i, :]), [yT], [k.mixT])
            S.barrier()


def mixer_gdn(k, l):
    S = k.S
    rot4 = Rot(k.PS[0:4])
    pks, pv, po, pS = k.PS[4], k.PS[5], k.PS[6], k.PS[7]
    load_conv_params(k, l)
    with contextlib.ExitStack() as st0:
        gn = S.sbuf("gdgn", [128, DEPTH], F32, st0)
        k.lvl = S.sbuf("gdlvl", [128, 7, 128], F32, st0)
        S.dma("sp", I("dma_start", out=k.lvl[:], in_=k.c_lvl[:]), [k.c_lvl], [k.lvl])
        S.dma("sp", I("dma_start", out=gn[:], in_=k.gdn_normT[:]), [k.gdn_normT], [gn])
        for h in range(6):
            with contextlib.ExitStack() as st:
                qT = S.sbuf("gdq", [128, L], BF16, st)
                kT = S.sbuf("gdk", [128, L], BF16, st)
                kTM = S.sbuf("gdkTM", [128, NCK, 128], BF16, st)
                kTf = S.sbuf("gdkTf", [128, L], F32, st)
                vTM = S.sbuf("gdvTM", [128, NCK, 128], BF16, st)
                zs = S.sbuf("gdzs", [128, L], F32, st)
                oacc = S.sbuf("gdo", [128, L], F32, st)
                with contextlib.ExitStack() as sta:
                    T = conv_temps(k, sta, l)
                    qf = S.sbuf("gdqf", [128, L], F32, sta)
                    vf = S.sbuf("gdvf", [128, L], BF16, sta)
                    sqr = Rot([S.sbuf(f"gdsq{i}", [128, 512], F32, sta) for i in range(2)])
                    rr = Rot([S.sbuf(f"gdrr{i}", [128, 512], F32, sta) for i in range(2)])
                    for ci, dst, scl in ((h, qT, 128 ** -0.5), (6 + h, kT, 1.0)):
                        conv_chunk(k, T, l, ci, qf[:], qf, rot4)
                        for t in range(4):
                            sq = sqr.next()
                            r = rr.next()
                            S.op("act", I("activation", out=sq[:], in_=qf[:, t * 512:(t + 1) * 512], func=AF.Square), [qf], [sq])
                            ps = rot4.next()
                            mm(S, ps[:], k.onesF[:], sq[:], True, True, [k.onesF, sq], [ps])
                            S.op("act", I("activation", out=r[:], in_=ps[:], func=AF.Sqrt, bias=k.epsT[:], scale=1.0), [ps, k.epsT], [r])
                            S.op("dve", I("reciprocal", out=r[:], in_=r[:]), [r], [r])
                            S.op("dve", I("scalar_tensor_tensor", out=dst[:, t * 512:(t + 1) * 512], in0=qf[:, t * 512:(t + 1) * 512], scalar=scl, in1=r[:],
                                          op0=ALU.mult, op1=ALU.mult), [qf, r], [dst])
                            if dst is kT:
                                S.op("pool", I("tensor_tensor", out=kTf[:, t * 512:(t + 1) * 512], in0=qf[:, t * 512:(t + 1) * 512], in1=r[:], op=ALU.mult), [qf, r], [kTf])
                    conv_chunk(k, T, l, 12 + h, vf[:], vf, rot4)
                    for src, dstm in ((vf, vTM), (kT, kTM)):
                        for n in range(NCK):
                            if n % 4 == 0:
                                pt = rot4.next()
                                ptb = pt[:].bitcast(BF16)
                            tr(S, ptb[:, (n % 4) * 128:(n % 4 + 1) * 128], src[:, n * 128:(n + 1) * 128], k.identB[:], [src, k.identB], [pt])
                            if n % 4 == 3:
                                evac(S, n // 4, dstm[:, n - 3:n + 1, :].rearrange("p a b -> p (a b)"), ptb[:, 0:512], [pt], [dstm])
                    w = T["w"].next()
                    S.dma("pool", I("dma_start", out=w[:], in_=k.w_in[l, :, C_GZ + h * 128:C_GZ + (h + 1) * 128].rearrange("(m p) c -> p m c", p=128)), [k.w_in], [w])
                    for t in range(4):
                        ps = rot4.next()
                        for kk in range(NK):
                            mm(S, ps[:], w[:, kk, :], k.hT[:, kk, t * 512:(t + 1) * 512], kk == 0, kk == NK - 1, [w, k.hT], [ps])
                        S.op("act", I("activation", out=zs[:, t * 512:(t + 1) * 512], in_=ps[:], func=AF.Silu), [ps], [zs])
                    S.barrier()
                for d in range(2):
                    if k.gstage < 2:
                        break
                    with contextlib.ExitStack() as stb:
                        T = decay_temps(k, stb)
                        QS = [S.sbuf(f"gdQS{i}", [128, 512], BF16, stb) for i in range(4)]
                        QC = [S.sbuf(f"gdQC{i}", [128, 512], BF16, stb) for i in range(4)]
                        TT = [S.sbuf(f"gdTT{i}", [128, 512], BF16, stb) for i in range(4)]
                        TC = [S.sbuf(f"gdTC{i}", [128, 512], BF16, stb) for i in range(4)]
                        qmr = Rot([S.sbuf(f"gdqm{i}", [128, 512], BF16, stb) for i in range(2)])
                        zr = Rot([S.sbuf(f"gdz{i}", [128, 512], BF16, stb) for i in range(2)])
                        qkm = S.sbuf("gdqkm", [128, L], BF16, stb)
                        qd = S.sbuf("gdqd", [128, L], BF16, stb)
                        bt = S.sbuf("gdbt", [128, NCK], F32, stb)
                        nbt = S.sbuf("gdnbt", [128, NCK], F32, stb)
                        negegc = S.sbuf("gdneg", [128, NCK], F32, stb)
                        bekd = S.sbuf("gdbekd", [128, NCK], F32, stb)
                        nstrict = S.sbuf("gdnst", [128, 128], F32, stb)
                        S32 = S.sbuf("gdS32", [128, 128], F32, stb)
                        Sbf = S.sbuf("gdSbf", [128, 128], BF16, stb)
                        tmp = Rot([S.sbuf(f"gdtmp{i}", [128, 512], F32, stb) for i in range(2)])
                        Ur = Rot([S.sbuf(f"gdU{i}", [128, 128], BF16, stb) for i in range(2)])
                        vnr = Rot([S.sbuf(f"gdvn{i}", [128, 128], BF16, stb) for i in range(2)])
                        vsr = Rot([S.sbuf(f"gdvs{i}", [128, 128], BF16, stb) for i in range(2)])
                        col = d * 6 + h
                        decay_prep(k, T, k.gdec[:, :, col], d, rot4)
                        ET = T["ET"]
                        valid = k.mup if d == 0 else k.mlow
                        S.op("dve", I("tensor_copy", out=bt[:], in_=k.beta[:, :, col]), [k.beta], [bt])
                        S.op("dve", I("tensor_scalar", out=nbt[:], in0=bt[:], scalar1=-1.0, scalar2=None, op0=ALU.mult), [bt], [nbt])
                        S.op("dve", I("tensor_scalar", out=negegc[:], in0=T["egc"][:], scalar1=-1.0, scalar2=None, op0=ALU.mult), [T["egc"]], [negegc])
                        S.op("dve", I("tensor_tensor", out=bekd[:], in0=bt[:], in1=T["ekd"][:], op=ALU.mult), [bt, T["ekd"]], [bekd])
                        S.op("dve", I("tensor_tensor", out=nstrict[:], in0=valid[:], in1=k.identF[:], op=ALU.subtract), [valid, k.identF], [nstrict])
                        S.op("pool", I("tensor_tensor", out=T["R"][:].rearrange("p (n s) -> p n s", s=CH), in0=k.identF[:].unsqueeze(1).broadcast_to([128, NCK, CH]),
                                       in1=T["egc"][:].unsqueeze(2).broadcast_to([128, NCK, CH]), op=ALU.mult), [k.identF, T["egc"]], [T["R"]])
                        for t in range(4):
                            pb = rot4.next()
                            mm(S, pb[:], k.onesF[:], T["R"][:, t * 512:(t + 1) * 512], True, True, [k.onesF, T["R"]], [pb])
                            S.op("dve", I("tensor_tensor", out=qd[:, t * 512:(t + 1) * 512], in0=pb[:], in1=qT[:, t * 512:(t + 1) * 512], op=ALU.mult), [pb, qT], [qd])
                            S.op("pool", I("tensor_scalar", out=qd[:, t * 512:(t + 1) * 512], in0=qd[:, t * 512:(t + 1) * 512], scalar1=1.0, scalar2=1.0, op0=ALU.add, op1=ALU.subtract), [qd], [qd])
                        for t in range(4):
                            pk = rot4.next()
                            pq = rot4.next()
                            for i in range(4):
                                n = t * 4 + i
                                mm(S, pk[:, i * 128:(i + 1) * 128], kT[:, n * 128:(n + 1) * 128], kT[:, n * 128:(n + 1) * 128], True, True, [kT], [pk])
                                mm(S, pq[:, i * 128:(i + 1) * 128], kT[:, n * 128:(n + 1) * 128], qT[:, n * 128:(n + 1) * 128], True, True, [kT, qT], [pq])
                            tm = tmp.next()
                            S.op("dve", I("tensor_tensor", out=tm[:], in0=pk[:], in1=ET[:, t * 512:(t + 1) * 512], op=ALU.mult), [pk, ET], [tm])
                            S.op("pool", I("tensor_tensor", out=tm[:].rearrange("p (n s) -> p n s", s=CH), in0=tm[:].rearrange("p (n s) -> p n s", s=CH),
                                           in1=nstrict[:].unsqueeze(1).broadcast_to([128, 4, CH]), op=ALU.mult), [tm, nstrict], [tm])
                            S.op("dve", I("tensor_tensor", out=QS[t][:].rearrange("p (n s) -> p n s", s=CH), in0=tm[:].rearrange("p (n s) -> p n s", s=CH),
                                          in1=nbt[:, t * 4:(t + 1) * 4].unsqueeze(2).broadcast_to([128, 4, CH]), op=ALU.mult), [tm, nbt], [QS[t]])
                            S.op("dve", I("tensor_tensor", out=qkm[:, t * 512:(t + 1) * 512], in0=pq[:], in1=ET[:, t * 512:(t + 1) * 512], op=ALU.mult), [pq, ET], [qkm])
                            pt = rot4.next()
                            ptb = pt[:].bitcast(BF16)
                            for i in range(4):
                                tr(S, ptb[:, i * 128:(i + 1) * 128], QS[t][:, i * 128:(i + 1) * 128], k.identB[:], [QS[t], k.identB], [pt])
                            S.op("act", I("activation", out=QC[t][:], in_=ptb[:, 0:512], func=AF.Copy), [pt], [QC[t]])
                        for t in range(4):
                            for X in (TT[t], TC[t]):
                                S.op("pool", I("tensor_copy", out=X[:].rearrange("p (n s) -> p n s", s=CH),
                                               in_=k.identB[:].unsqueeze(1).broadcast_to([128, 4, CH])), [k.identB], [X])
                        for lev in range(7 if k.gstage >= 3 else 0):
                            for t in range(4):
                                qm = qmr.next()
                                S.op("pool", I("tensor_tensor", out=qm[:].rearrange("p (n s) -> p n s", s=CH), in0=QC[t][:].rearrange("p (n s) -> p n s", s=CH),
                                               in1=k.lvl[:, lev, :].unsqueeze(1).broadcast_to([128, 4, CH]), op=ALU.mult), [QC[t], k.lvl], [qm])
                                pz = rot4.next()
                                for i in range(4):
                                    sl = slice(i * 128, (i + 1) * 128)
                                    mm(S, pz[:, sl], qm[:, sl], TT[t][:, sl], True, True, [qm, TT[t]], [pz])
                                z = zr.next()
                                S.op("dve", I("tensor_scalar", out=z[:], in0=pz[:], scalar1=1.0, scalar2=1.0, op0=ALU.add, op1=ALU.subtract), [pz], [z])
                                pw = rot4.next()
                                pwt = rot4.next()
                                for i in range(4):
                                    sl = slice(i * 128, (i + 1) * 128)
                                    mm(S, pw[:, sl], TC[t][:, sl], z[:, sl], True, True, [TC[t], z], [pw])
                                    mm(S, pwt[:, sl], z[:, sl], TC[t][:, sl], True, True, [TC[t], z], [pwt])
                                S.op("dve", I("tensor_tensor", out=TT[t][:], in0=pw[:], in1=TT[t][:], op=ALU.add), [pw, TT[t]], [TT[t]])
                                S.op("pool", I("tensor_scalar", out=TT[t][:], in0=TT[t][:], scalar1=1.0, scalar2=1.0, op0=ALU.add, op1=ALU.subtract), [TT[t]], [TT[t]])
                                S.op("dve", I("tensor_tensor", out=TC[t][:], in0=pwt[:], in1=TC[t][:], op=ALU.add), [pwt, TC[t]], [TC[t]])
                                S.op("pool", I("tensor_scalar", out=TC[t][:], in0=TC[t][:], scalar1=1.0, scalar2=1.0, op0=ALU.add, op1=ALU.subtract), [TC[t]], [TC[t]])
                        S.op("dve", I("memset", S32[:], 0.0), [], [S32])
                        S.op("dve", I("memset", Sbf[:], 0.0), [], [Sbf])
                        order = list(range(NCK)) if d == 0 else list(range(NCK - 1, -1, -1))
                        for idx, n in enumerate(order if k.gstage >= 4 else []):
                            cs = slice(n * 128, (n + 1) * 128)
                            t, i = n // 4, n % 4
                            mm(S, pks[:, 0:128], kTf[:, cs], S32[:], True, True, [kTf, S32], [pks])
                            U = Ur.next()
                            S.op("dve", I("scalar_tensor_tensor", out=U[:], in0=pks[:, 0:128], scalar=negegc[:, n:n + 1], in1=vTM[:, n, :], op0=ALU.mult, op1=ALU.add), [pks, negegc, vTM], [U])
                            S.op("pool", I("tensor_scalar", out=U[:], in0=U[:], scalar1=1.0, scalar2=1.0, op0=ALU.add, op1=ALU.subtract), [U], [U])
                            mm(S, pv[:, 0:128], TT[t][:, i * 128:(i + 1) * 128], U[:], True, True, [TT[t], U], [pv])
                            vn = vnr.next()
                            vs = vsr.next()
                            S.op("dve", I("tensor_scalar", out=vn[:], in0=pv[:, 0:128], scalar1=bt[:, n:n + 1], scalar2=None, op0=ALU.mult), [pv, bt], [vn])
                            S.op("dve", I("tensor_scalar", out=vs[:], in0=pv[:, 0:128], scalar1=bekd[:, n:n + 1], scalar2=None, op0=ALU.mult), [pv, bekd], [vs])
                            S.op("pool", I("tensor_scalar", out=vn[:], in0=vn[:], scalar1=1.0, scalar2=1.0, op0=ALU.add, op1=ALU.subtract), [vn], [vn])
                            S.op("pool", I("tensor_scalar", out=vs[:], in0=vs[:], scalar1=1.0, scalar2=1.0, op0=ALU.add, op1=ALU.subtract), [vs], [vs])
                            osl = slice(i * 128, (i + 1) * 128)
                            mm(S, po[:, osl], Sbf[:], qd[:, cs], True, False, [Sbf, qd], [po])
                            mm(S, po[:, osl], vn[:], qkm[:, cs], False, True, [vn, qkm], [po])
                            mm(S, pS[:, 0:128], kTM[:, n, :], vs[:], True, True, [kTM, vs], [pS])
                            S.op("dve", I("scalar_tensor_tensor", out=S32[:], in0=S32[:], scalar=T["egl"][:, n:n + 1], in1=pS[:, 0:128], op0=ALU.mult, op1=ALU.add), [S32, T["egl"], pS], [S32])
                            S.op("pool", I("tensor_scalar", out=S32[:], in0=S32[:], scalar1=1.0, scalar2=1.0, op0=ALU.add, op1=ALU.subtract), [S32], [S32])
                            S.op("act", I("activation", out=Sbf[:], in_=S32[:], func=AF.Copy), [S32], [Sbf])
                            if idx % 4 == 3:
                                ts_ = slice(t * 512, (t + 1) * 512)
                                if d == 0:
                                    S.op("act", I("activation", out=oacc[:, ts_], in_=po[:], func=AF.Copy), [po], [oacc])
                                else:
                                    S.op("dve", I("tensor_tensor", out=oacc[:, ts_], in0=po[:], in1=oacc[:, ts_], op=ALU.add), [po, oacc], [oacc])
                        S.barrier()
                with contextlib.ExitStack() as stc:
                    sqr = Rot([S.sbuf(f"gdsq2{i}", [128, 512], F32, stc) for i in range(2)])
                    rr = Rot([S.sbuf(f"gdrr2{i}", [128, 512], F32, stc) for i in range(2)])
                    yb = S.sbuf("gdyb", [128, L], BF16, stc)
                    for t in range(4):
                        ts_ = slice(t * 512, (t + 1) * 512)
                        sq = sqr.next()
                        r = rr.next()
                        S.op("act", I("activation", out=sq[:], in_=oacc[:, ts_], func=AF.Square), [oacc], [sq])
                        ps = rot4.next()
                        mm(S, ps[:], k.onesF[:], sq[:], True, True, [k.onesF, sq], [ps])
                        S.op("act", I("activation", out=r[:], in_=ps[:], func=AF.Sqrt, bias=k.epsT[:], scale=1.0 / 128), [ps, k.epsT], [r])
                        S.op("dve", I("reciprocal", out=r[:], in_=r[:]), [r], [r])
                        S.op("dve", I("tensor_tensor", out=r[:], in0=r[:], in1=oacc[:, ts_], op=ALU.mult), [r, oacc], [r])
                        S.op("dve", I("scalar_tensor_tensor", out=yb[:, ts_], in0=r[:], scalar=gn[:, l:l + 1], in1=zs[:, ts_], op0=ALU.mult, op1=ALU.mult), [r, gn, zs], [yb])
                    S.dma("sp", I("dma_start", out=k.mixT[h * 128:(h + 1) * 128, :], in_=yb[:]), [yb], [k.mixT])
                    S.barrier()


_CACHE = {}


def _consts():
    p = np.arange(128)
    c = {}
    c["c_identF"] = np.eye(128, dtype=np.float32)
    c["c_mlow"] = (p[:, None] >= p[None, :]).astype(np.float32)
    c["c_mup"] = (p[None, :] >= p[:, None]).astype(np.float32)
    cols = np.arange(64)
    cs = np.clip(cols - 8, 0, 48)
    inwin = (cols[None, :] >= cs[:, None]) & (cols[None, :] < cs[:, None] + 16)
    c["c_namask"] = np.where(inwin, 0.0, -30000.0).astype(np.float32)
    sel = np.zeros((8, 8, 128), np.float32)
    for e in range(8):
        sel[e, e, :] = 1.0
    c["c_sel"] = sel
    lv = np.zeros((128, 7, 128), np.float32)
    for i in range(7):
        bsz = 1 << i
        lv[:, i, :] = ((p[:, None] // (2 * bsz) == p[None, :] // (2 * bsz)) & (p[:, None] // bsz != p[None, :] // bsz))
    c["c_lvl"] = lv
    return c


def _layout(inp, b):
    f = np.float32
    g = {}
    g["x"] = np.ascontiguousarray(inp["x"][b])
    g["cT"] = np.ascontiguousarray(inp["c"][b].reshape(NK, 128).T)
    g["ada_bT"] = np.ascontiguousarray(inp["ada_b"].reshape(DEPTH, 96, 128).transpose(2, 0, 1))
    g["nmixT"] = np.ascontiguousarray(inp["norm_mix"].reshape(DEPTH, NK, 128).transpose(2, 0, 1))
    g["nffnT"] = np.ascontiguousarray(inp["norm_ffn"].reshape(DEPTH, NK, 128).transpose(2, 0, 1))
    g["nfinT"] = np.ascontiguousarray(inp["norm_final"].reshape(NK, 128).T)
    g["conv_wT"] = np.ascontiguousarray(inp["conv_w"].reshape(DEPTH, 5, 28, 128).transpose(3, 0, 2, 1))
    g["conv_bT"] = np.ascontiguousarray(inp["conv_b"].reshape(DEPTH, 28, 128).transpose(2, 0, 1))
    gb = np.zeros((DEPTH, 48), f)
    gb[:, 12:24] = inp["gdn_dt_bias"].reshape(DEPTH, 12)
    gb[:, 24:48] = inp["ssm_dt_bias"].reshape(DEPTH, 24)
    ga = np.zeros((DEPTH, 48), f)
    ga[:, 12:24] = inp["gdn_A_log"].reshape(DEPTH, 12)
    ga[:, 24:48] = inp["ssm_A_log"].reshape(DEPTH, 24)
    g["gate_bias"] = np.ascontiguousarray(np.broadcast_to(gb[None], (128, DEPTH, 48)))
    g["gate_alog"] = np.ascontiguousarray(np.broadcast_to(ga[None], (128, DEPTH, 48)))
    g["gdn_normT"] = np.ascontiguousarray(inp["gdn_norm"].T)
    g["ssm_Dbc"] = np.ascontiguousarray(np.broadcast_to(inp["ssm_D"][None], (128, DEPTH, 12)))
    g["ssm_normbc"] = np.ascontiguousarray(np.broadcast_to(inp["ssm_norm"][None], (128, DEPTH, 768)))
    cols = np.arange(64)
    dc = np.clip(cols[None, :] - cols[:, None], -15, 15) + 15
    rpb = inp["na_rpb"]
    g["na_rb"] = np.ascontiguousarray(rpb[:, :, :, dc].transpose(0, 1, 3, 2, 4).reshape(DEPTH, 8, 64, 15 * 64))
    for name in ("ada_w", "w_in", "w_out", "ffn_w1", "ffn_w3", "ffn_w2", "moe_router", "moe_w1", "moe_w3", "moe_w2"):
        g[name] = inp[name]
    g.update(_consts())
    return g


def kernel(**inputs):
    inp = {n: np.asarray(v, dtype=np.float32) for n, v in inputs.items()}
    if "nc" not in _CACHE:
        _CACHE["nc"] = build()
    nc = _CACHE["nc"]
    shared = None
    in_maps = []
    for b in range(8):
        m = _layout(inp, b) if shared is None else dict(shared)
        if shared is None:
            shared = m
        else:
            m["x"] = np.ascontiguousarray(inp["x"][b])
            m["cT"] = np.ascontiguousarray(inp["c"][b].reshape(NK, 128).T)
        in_maps.append(m)
    res = run_bass_kernel_spmd(nc, in_maps, core_ids=list(range(8)))
    return np.stack([res.results[b]["out"] for b in range(8)], axis=0).astype(np.float32)
```
